# Optimizing a Trainium2 kernel written in Bass

```python
import jax
import jax.numpy as jnp
from jax import lax
import numpy as np

D_MODEL = 1024
BATCH = 8
SEQ = 4096
DEPTH = 4

GRID_W = 64
CTX_LEN = 256
NORM_EPS = 1e-6
ROPE_THETA = 10000.0

ATT_HEADS = 8
ATT_KV_HEADS = 2
ATT_HEAD_DIM = 64
Q_BLOCK = 128
ATT_W = ATT_HEADS * ATT_HEAD_DIM
ATT_KV_W = ATT_KV_HEADS * ATT_HEAD_DIM

RET_HEADS = 4
RET_HEAD_DIM = 128
RET_CHUNK = 128
RET_W = RET_HEADS * RET_HEAD_DIM

RWKV_HEADS = 8
RWKV_HEAD_DIM = 64
RWKV_W = RWKV_HEADS * RWKV_HEAD_DIM
RWKV_DECAY_LORA = 64
RWKV_AAA_LORA = 64
RWKV_GATE_LORA = 128
RWKV_GN_EPS = 64e-5
RWKV_SPLITS = (RWKV_W, RWKV_W, RWKV_W, RWKV_DECAY_LORA, RWKV_AAA_LORA, RWKV_GATE_LORA)
RWKV_COLS = 3 * RWKV_W + RWKV_DECAY_LORA + RWKV_AAA_LORA + RWKV_GATE_LORA

N_BRANCH = 3
BRANCH_W = 512
IN_SPLITS = (ATT_W, ATT_KV_W, ATT_KV_W, RET_W, RET_W, RET_W, RET_W, RWKV_COLS, N_BRANCH * D_MODEL)
IN_COLS = ATT_W + 2 * ATT_KV_W + 4 * RET_W + RWKV_COLS + N_BRANCH * D_MODEL

MOE_GROUPS = 4
MOE_EXPERTS_PER_GROUP = 8
MOE_EXPERTS = MOE_GROUPS * MOE_EXPERTS_PER_GROUP
MOE_TOPK = 2
MOE_HIDDEN = 512
MOE_BLOCK = 256

kernel_name = 'hybrid_prefix_dit_block'


def rms_norm(x, g):
    xf = x.astype(jnp.float32)
    y = xf * lax.rsqrt(jnp.mean(xf * xf, axis=-1, keepdims=True) + NORM_EPS)
    return (y * g.astype(jnp.float32)).astype(x.dtype)


def split_cols(u, sizes):
    bounds = []
    acc = 0
    for s in sizes[:-1]:
        acc += s
        bounds.append(acc)
    return jnp.split(u, bounds, axis=-1)


def split_heads(u, n_heads):
    b, n, w = u.shape
    return u.reshape(b, n, n_heads, w // n_heads).transpose(0, 2, 1, 3)


def merge_heads(u):
    b, h, n, d = u.shape
    return u.transpose(0, 2, 1, 3).reshape(b, n, h * d)


def axial_rope_tables(n_tokens, head_dim):
    rows = n_tokens // GRID_W
    row = jnp.broadcast_to(jnp.arange(rows, dtype=jnp.float32)[:, None], (rows, GRID_W)).reshape(-1)
    col = jnp.broadcast_to(jnp.arange(GRID_W, dtype=jnp.float32)[None, :], (rows, GRID_W)).reshape(-1)
    quarter = head_dim // 4
    inv_freq = ROPE_THETA ** (-jnp.arange(quarter, dtype=jnp.float32) / quarter)
    ang = jnp.stack([row[:, None] * inv_freq, col[:, None] * inv_freq], axis=1)
    return jnp.cos(ang), jnp.sin(ang)


def apply_axial_rope(x, cos, sin):
    shape = x.shape
    xr = x.astype(jnp.float32).reshape(*shape[:-1], 2, 2, shape[-1] // 4)
    x1, x2 = xr[..., 0, :], xr[..., 1, :]
    out = jnp.stack([x1 * cos - x2 * sin, x2 * cos + x1 * sin], axis=-2)
    return out.reshape(shape).astype(x.dtype)


def centred_shift(u, mu):
    prev = jnp.pad(u[:, :-1], ((0, 0), (1, 0), (0, 0)))
    nxt = jnp.pad(u[:, 1:], ((0, 0), (0, 1), (0, 0)))
    return u + (prev - u) * mu[0] + (nxt - u) * mu[1]


def gqa_softmax(q, k, v):
    s = jnp.einsum('bkgqd,bksd->bkgqs', q.astype(jnp.float32), k) * (q.shape[-1] ** -0.5)
    return jnp.einsum('bkgqs,bksd->bkgqd', jax.nn.softmax(s, axis=-1), v)


def attention_branch(q, k, v, qc, kc, vc, qn_g, kn_g, cos, sin, want_ctx):
    b, n, _ = q.shape
    n_ctx = qc.shape[1]
    grp = ATT_HEADS // ATT_KV_HEADS
    qh = apply_axial_rope(rms_norm(split_heads(q, ATT_HEADS), qn_g), cos, sin)
    kh = apply_axial_rope(rms_norm(split_heads(k, ATT_KV_HEADS), kn_g), cos, sin)
    kch = rms_norm(split_heads(kc, ATT_KV_HEADS), kn_g).astype(jnp.float32)
    vch = split_heads(vc, ATT_KV_HEADS).astype(jnp.float32)
    keys = jnp.concatenate([kch, kh.astype(jnp.float32)], axis=2)
    vals = jnp.concatenate([vch, split_heads(v, ATT_KV_HEADS).astype(jnp.float32)], axis=2)
    qb = qh.reshape(b, ATT_KV_HEADS, grp, n // Q_BLOCK, Q_BLOCK, ATT_HEAD_DIM).transpose(3, 0, 1, 2, 4, 5)
    ob = lax.map(lambda qi: gqa_softmax(qi, keys, vals), qb)
    out = ob.transpose(1, 2, 3, 0, 4, 5).reshape(b, ATT_HEADS, n, ATT_HEAD_DIM)
    y = merge_heads(out).astype(q.dtype)
    if not want_ctx:
        return y, None
    qch = rms_norm(split_heads(qc, ATT_HEADS), qn_g).reshape(b, ATT_KV_HEADS, grp, n_ctx, ATT_HEAD_DIM)
    oc = gqa_softmax(qch, kch, vch).reshape(b, ATT_HEADS, n_ctx, ATT_HEAD_DIM)
    return y, merge_heads(oc).astype(qc.dtype)


def chunk_retention(q, k, v, log_gamma, s0, want_out):
    bsz, nh, n, dk = k.shape
    dv = v.shape[-1]
    n_chunks = n // RET_CHUNK
    pos = jnp.arange(RET_CHUNK, dtype=jnp.float32)
    d_key = jnp.exp(log_gamma[:, None] * (RET_CHUNK - 1.0 - pos))[:, :, None]
    d_chunk = jnp.exp(log_gamma * RET_CHUNK)[:, None, None]
    if want_out:
        lag = pos[:, None] - pos[None, :]
        d_intra = jnp.where(lag >= 0, jnp.exp(log_gamma[:, None, None] * jnp.maximum(lag, 0.0)), 0.0)
        d_query = jnp.exp(log_gamma[:, None] * (pos + 1.0))[:, :, None]
    to_chunks = lambda u: u.reshape(bsz, nh, n_chunks, RET_CHUNK, u.shape[-1]).transpose(2, 0, 1, 3, 4)

    def step(s, blk):
        qc, kc, vc = blk
        s_next = d_chunk * s + jnp.einsum('bhjd,bhje->bhde', kc * d_key, vc)
        if not want_out:
            return s_next, None
        scores = jnp.einsum('bhid,bhjd->bhij', qc, kc) * d_intra
        y = jnp.einsum('bhij,bhje->bhie', scores, vc) + jnp.einsum('bhid,bhde->bhie', qc, s) * d_query
        return s_next, y

    xs = (to_chunks(q) if want_out else None, to_chunks(k), to_chunks(v))
    s_fin, ys = lax.scan(step, s0, xs)
    if not want_out:
        return s_fin, None
    return s_fin, ys.transpose(1, 2, 0, 3, 4).reshape(bsz, nh, n, dv)


def retention_branch(q, k, v, g, qc, kc, vc, gc, decay_logit, gn_g, cos, sin, want_ctx):
    f32 = jnp.float32
    scale = RET_HEAD_DIM ** -0.5
    log_gamma = jax.nn.log_sigmoid(decay_logit.astype(f32))
    ql = apply_axial_rope(split_heads(q, RET_HEADS), cos, sin).astype(f32)
    kl = apply_axial_rope(split_heads(k, RET_HEADS), cos, sin).astype(f32) * scale
    vl = split_heads(v, RET_HEADS).astype(f32)
    qcx = split_heads(qc, RET_HEADS).astype(f32) if want_ctx else None
    kcx = split_heads(kc, RET_HEADS).astype(f32) * scale
    vcx = split_heads(vc, RET_HEADS).astype(f32)
    s0 = jnp.zeros((kcx.shape[0], RET_HEADS, RET_HEAD_DIM, RET_HEAD_DIM), f32)
    flip = lambda u: None if u is None else jnp.flip(u, axis=2)
    s_cf, y_cf = chunk_retention(qcx, kcx, vcx, log_gamma[0], s0, want_ctx)
    s_cb, y_cb = chunk_retention(flip(qcx), flip(kcx), flip(vcx), log_gamma[1], s0, want_ctx)
    _, y_lf = chunk_retention(ql, kl, vl, log_gamma[0], s_cf, True)
    _, y_lb = chunk_retention(flip(ql), flip(kl), flip(vl), log_gamma[1], s_cb, True)

    def readout(y, gate):
        yn = rms_norm(y, gn_g.reshape(RET_HEADS, 1, RET_HEAD_DIM))
        return (jax.nn.silu(gate.astype(f32)) * merge_heads(yn)).astype(gate.dtype)

    y = readout(y_lf + flip(y_lb), g)
    if not want_ctx:
        return y, None
    return y, readout(y_cf + flip(y_cb), gc)


def rwkv7_scan(r, decay, k, v, a_in, b_in, s0, reverse, want_out):
    time_major = lambda u: None if u is None else jnp.swapaxes(u, 0, 1)

    def step(s, inp):
        rt, wt, kt, vt, at, bt = inp
        sa = jnp.einsum('bhij,bhj->bhi', s, at)
        s = s * wt[:, :, None, :] + sa[..., None] * bt[:, :, None, :] + vt[..., None] * kt[:, :, None, :]
        return s, (jnp.einsum('bhij,bhj->bhi', s, rt) if want_out else None)

    xs = tuple(time_major(u) for u in (r, decay, k, v, a_in, b_in))
    s_fin, ys = lax.scan(step, s0, xs, reverse=reverse)
    return s_fin, (jnp.swapaxes(ys, 0, 1) if want_out else None)


def rwkv7_branch(u, uc, mu, w0, w2, a0, a2, g2, k_k, k_a, r_k, ln_g, ln_b, want_ctx):
    f32 = jnp.float32

    def scan_inputs(t):
        bsz, n, _ = t.shape
        t = centred_shift(t.astype(f32), mu.astype(f32))
        r, k, v, xw, xa, xg = split_cols(t, RWKV_SPLITS)
        heads = lambda z: z.reshape(bsz, n, RWKV_HEADS, RWKV_HEAD_DIM)
        kk = heads(k * k_k)
        kk = kk * lax.rsqrt(jnp.maximum(jnp.sum(kk * kk, axis=-1, keepdims=True), 1e-12))
        dirs = []
        for d in range(2):
            w_log = -jax.nn.softplus(-(w0[d] + jnp.tanh(xw) @ w2[d])) - 0.5
            a = jax.nn.sigmoid(a0[d] + xa @ a2[d])
            dirs.append((heads(jnp.exp(-jnp.exp(w_log))),
                         heads(k * (1.0 + (a - 1.0) * k_a)),
                         kk * heads(a)))
        return heads(r), heads(v), kk, xg, dirs

    def readout(y, r, v, dirs, xg):
        bsz, n = y.shape[:2]
        mean = jnp.mean(y, axis=-1, keepdims=True)
        var = jnp.mean(jnp.square(y - mean), axis=-1, keepdims=True)
        yn = ((y - mean) * lax.rsqrt(var + RWKV_GN_EPS)).reshape(bsz, n, RWKV_W) * ln_g + ln_b
        bonus = jnp.sum(r * (dirs[0][1] + dirs[1][1]) * r_k, axis=-1, keepdims=True) * v
        return (yn + bonus.reshape(bsz, n, RWKV_W)) * (jax.nn.sigmoid(xg) @ g2)

    r, v, kk, xg, dirs = scan_inputs(u)
    rc, vc, kkc, xgc, dirs_c = scan_inputs(uc)
    s0 = jnp.zeros((u.shape[0], RWKV_HEADS, RWKV_HEAD_DIM, RWKV_HEAD_DIM), f32)
    y_lat, y_ctx = [], []
    for d in range(2):
        s_ctx, yc = rwkv7_scan(rc if want_ctx else None, dirs_c[d][0], dirs_c[d][1], vc, -kkc, dirs_c[d][2],
                               s0, d == 1, want_ctx)
        _, yl = rwkv7_scan(r, dirs[d][0], dirs[d][1], v, -kk, dirs[d][2], s_ctx, d == 1, True)
        y_lat.append(yl)
        y_ctx.append(yc)
    out = readout(y_lat[0] + y_lat[1], r, v, dirs, xg).astype(u.dtype)
    if not want_ctx:
        return out, None
    return out, readout(y_ctx[0] + y_ctx[1], rc, vc, dirs_c, xgc).astype(uc.dtype)


def merge_branches(y_att, y_ret, y_rwkv, gate_logits, w_branch, w_out):
    ys = jnp.stack([y_att, y_ret, y_rwkv], axis=-2)
    proj = jnp.einsum('blnw,nwd->blnd', ys, w_branch)
    gates = jax.nn.sigmoid(gate_logits.astype(jnp.float32)).reshape(proj.shape)
    return jnp.sum(gates * proj, axis=-2).astype(w_out.dtype) @ w_out


def hybrid_mixer(h, hc, w_in, att_qn_g, att_kn_g, ret_decay_logit, ret_gn_g, rwkv_mu, rwkv_w0, rwkv_w2,
                 rwkv_a0, rwkv_a2, rwkv_g2, rwkv_k_k, rwkv_k_a, rwkv_r_k, rwkv_ln_g, rwkv_ln_b,
                 w_branch, w_out, att_cos, att_sin, ret_cos, ret_sin, want_ctx):
    aq, ak, av, rq, rk, rv, rg, wu, gl = split_cols(h @ w_in, IN_SPLITS)
    aqc, akc, avc, rqc, rkc, rvc, rgc, wuc, glc = split_cols(hc @ w_in, IN_SPLITS)
    ya, yac = attention_branch(aq, ak, av, aqc, akc, avc, att_qn_g, att_kn_g, att_cos, att_sin, want_ctx)
    yr, yrc = retention_branch(rq, rk, rv, rg, rqc, rkc, rvc, rgc, ret_decay_logit, ret_gn_g,
                               ret_cos, ret_sin, want_ctx)
    yw, ywc = rwkv7_branch(wu, wuc, rwkv_mu, rwkv_w0, rwkv_w2, rwkv_a0, rwkv_a2, rwkv_g2, rwkv_k_k,
                           rwkv_k_a, rwkv_r_k, rwkv_ln_g, rwkv_ln_b, want_ctx)
    out = merge_branches(ya, yr, yw, gl, w_branch, w_out).astype(h.dtype)
    if not want_ctx:
        return out, None
    return out, merge_branches(yac, yrc, ywc, glc, w_branch, w_out).astype(hc.dtype)


def hier_moe(t, grp_w, grp_b, exp_w, exp_b, w_gate, w_up, w_down):
    n_tok, d = t.shape
    tf = t.astype(jnp.float32)
    grp_logits = tf @ grp_w.astype(jnp.float32) + grp_b.astype(jnp.float32)
    grp = jnp.argmax(grp_logits, axis=-1)
    p_grp = jnp.max(jax.nn.softmax(grp_logits, axis=-1), axis=-1, keepdims=True)
    exp_logits = (tf @ exp_w.astype(jnp.float32) + exp_b.astype(jnp.float32)).reshape(
        n_tok, MOE_GROUPS, MOE_EXPERTS_PER_GROUP)
    in_grp = exp_logits[jnp.arange(n_tok), grp]
    top_val, top_idx = lax.top_k(in_grp, MOE_TOPK)
    weights = (jax.nn.softmax(top_val, axis=-1) * p_grp).reshape(-1)
    expert = (grp[:, None] * MOE_EXPERTS_PER_GROUP + top_idx).reshape(-1)
    n_pair = n_tok * MOE_TOPK
    order = jnp.argsort(expert)
    e_sorted = expert[order]
    counts = jnp.bincount(expert, length=MOE_EXPERTS)
    padded = (counts + MOE_BLOCK - 1) // MOE_BLOCK * MOE_BLOCK
    pad_end = jnp.cumsum(padded)
    pad_start = pad_end - padded
    grp_start = jnp.cumsum(counts) - counts
    dest = pad_start[e_sorted] + jnp.arange(n_pair) - grp_start[e_sorted]
    n_blocks = -(-n_pair // MOE_BLOCK) + MOE_EXPERTS
    buf = jnp.zeros((n_blocks * MOE_BLOCK, d), t.dtype).at[dest].set(t[order // MOE_TOPK])
    block_expert = jnp.minimum(jnp.searchsorted(pad_end, jnp.arange(n_blocks) * MOE_BLOCK, side='right'),
                               MOE_EXPERTS - 1)

    def expert_block(args):
        xb, e = args
        return (jax.nn.silu(xb @ w_gate[e]) * (xb @ w_up[e])) @ w_down[e]

    yb = lax.map(expert_block, (buf.reshape(n_blocks, MOE_BLOCK, d), block_expert)).reshape(-1, d)
    y_pairs = yb[dest].astype(jnp.float32) * weights[order][:, None]
    y = jnp.zeros((n_tok, d), jnp.float32).at[order // MOE_TOPK].add(y_pairs)
    return y.astype(t.dtype)


def setup_inputs(seed: int = 0) -> dict:
    key = jax.random.key(seed)
    ks = iter(jax.random.split(key, 40))
    f32 = jnp.float32
    nrm = lambda shape, scale: jax.random.normal(next(ks), shape, f32) * scale
    L, D = DEPTH, D_MODEL
    x = nrm((BATCH, SEQ, D), 1.0)
    c = nrm((BATCH, D), 1.0)
    ctx = nrm((BATCH, CTX_LEN, D), 1.0)
    c_ctx = nrm((D,), 1.0)
    ada_w = nrm((L, D, 6 * D), 0.5 * D ** -0.5)
    ada_b = nrm((L, 6 * D), 0.02)
    norm1_g = 1.0 + nrm((L, D), 0.02)
    norm2_g = 1.0 + nrm((L, D), 0.02)
    w_in = nrm((L, D, IN_COLS), D ** -0.5)
    att_qn_g = 1.0 + nrm((L, ATT_HEAD_DIM), 0.02)
    att_kn_g = 1.0 + nrm((L, ATT_HEAD_DIM), 0.02)
    ret_base = jnp.log(2.0 ** (5.0 + jnp.arange(RET_HEADS, dtype=f32)) - 1.0)
    ret_decay_logit = ret_base[None, None, :] + nrm((L, 2, RET_HEADS), 0.1)
    ret_gn_g = 1.0 + nrm((L, RET_W), 0.02)
    rwkv_mu = jax.random.uniform(next(ks), (L, 2, RWKV_COLS), f32, 0.05, 0.45)
    rwkv_w0 = jnp.linspace(-6.5, -1.5, RWKV_W, dtype=f32)[None, None, :] + nrm((L, 2, RWKV_W), 0.1)
    rwkv_w2 = nrm((L, 2, RWKV_DECAY_LORA, RWKV_W), 0.1)
    rwkv_a0 = nrm((L, 2, RWKV_W), 0.1)
    rwkv_a2 = nrm((L, 2, RWKV_AAA_LORA, RWKV_W), 0.3 * RWKV_AAA_LORA ** -0.5)
    rwkv_g2 = nrm((L, RWKV_GATE_LORA, RWKV_W), RWKV_GATE_LORA ** -0.5)
    rwkv_k_k = 0.85 + nrm((L, RWKV_W), 0.02)
    rwkv_k_a = 1.0 + nrm((L, RWKV_W), 0.02)
    rwkv_r_k = nrm((L, RWKV_HEADS, RWKV_HEAD_DIM), 0.1)
    rwkv_ln_g = 1.0 + nrm((L, RWKV_W), 0.02)
    rwkv_ln_b = nrm((L, RWKV_W), 0.02)
    w_branch = nrm((L, N_BRANCH, BRANCH_W, D), BRANCH_W ** -0.5)
    w_out = nrm((L, D, D), D ** -0.5)
    router_grp_w = nrm((L, D, MOE_GROUPS), D ** -0.5)
    router_grp_b = nrm((L, MOE_GROUPS), 0.01)
    router_exp_w = nrm((L, D, MOE_EXPERTS), D ** -0.5)
    router_exp_b = nrm((L, MOE_EXPERTS), 0.01)
    moe_w_gate = nrm((L, MOE_EXPERTS, D, MOE_HIDDEN), D ** -0.5)
    moe_w_up = nrm((L, MOE_EXPERTS, D, MOE_HIDDEN), D ** -0.5)
    moe_w_down = nrm((L, MOE_EXPERTS, MOE_HIDDEN, D), MOE_HIDDEN ** -0.5)
    return {'x': x, 'c': c, 'ctx': ctx, 'c_ctx': c_ctx, 'ada_w': ada_w, 'ada_b': ada_b,
            'norm1_g': norm1_g, 'norm2_g': norm2_g, 'w_in': w_in, 'att_qn_g': att_qn_g, 'att_kn_g': att_kn_g,
            'ret_decay_logit': ret_decay_logit, 'ret_gn_g': ret_gn_g, 'rwkv_mu': rwkv_mu, 'rwkv_w0': rwkv_w0,
            'rwkv_w2': rwkv_w2, 'rwkv_a0': rwkv_a0, 'rwkv_a2': rwkv_a2, 'rwkv_g2': rwkv_g2,
            'rwkv_k_k': rwkv_k_k, 'rwkv_k_a': rwkv_k_a, 'rwkv_r_k': rwkv_r_k, 'rwkv_ln_g': rwkv_ln_g,
            'rwkv_ln_b': rwkv_ln_b, 'w_branch': w_branch, 'w_out': w_out, 'router_grp_w': router_grp_w,
            'router_grp_b': router_grp_b, 'router_exp_w': router_exp_w, 'router_exp_b': router_exp_b,
            'moe_w_gate': moe_w_gate, 'moe_w_up': moe_w_up, 'moe_w_down': moe_w_down}


def reference(x, c, ctx, c_ctx, ada_w, ada_b, norm1_g, norm2_g, w_in, att_qn_g, att_kn_g,
              ret_decay_logit, ret_gn_g, rwkv_mu, rwkv_w0, rwkv_w2, rwkv_a0, rwkv_a2, rwkv_g2,
              rwkv_k_k, rwkv_k_a, rwkv_r_k, rwkv_ln_g, rwkv_ln_b, w_branch, w_out,
              router_grp_w, router_grp_b, router_exp_w, router_exp_b, moe_w_gate, moe_w_up, moe_w_down):
    b, n_lat, d = x.shape
    att_cos, att_sin = axial_rope_tables(n_lat, ATT_HEAD_DIM)
    ret_cos, ret_sin = axial_rope_tables(n_lat, RET_HEAD_DIM)
    cond = jax.nn.silu(c)
    cond_ctx = jax.nn.silu(c_ctx)
    for layer in range(DEPTH):
        want_ctx = layer < DEPTH - 1
        sh1, sc1, g1, sh2, sc2, g2 = jnp.split((cond @ ada_w[layer] + ada_b[layer])[:, None, :], 6, axis=-1)
        sh1c, sc1c, g1c, sh2c, sc2c, g2c = jnp.split(cond_ctx @ ada_w[layer] + ada_b[layer], 6, axis=-1)
        h = rms_norm(x, norm1_g[layer]) * (1.0 + sc1) + sh1
        hc = rms_norm(ctx, norm1_g[layer]) * (1.0 + sc1c) + sh1c
        m, mc = hybrid_mixer(h, hc, w_in[layer], att_qn_g[layer], att_kn_g[layer], ret_decay_logit[layer],
                             ret_gn_g[layer], rwkv_mu[layer], rwkv_w0[layer], rwkv_w2[layer], rwkv_a0[layer],
                             rwkv_a2[layer], rwkv_g2[layer], rwkv_k_k[layer], rwkv_k_a[layer], rwkv_r_k[layer],
                             rwkv_ln_g[layer], rwkv_ln_b[layer], w_branch[layer], w_out[layer],
                             att_cos, att_sin, ret_cos, ret_sin, want_ctx)
        x = x + g1 * m
        h2 = rms_norm(x, norm2_g[layer]) * (1.0 + sc2) + sh2
        tokens = h2.reshape(-1, d)
        if want_ctx:
            ctx = ctx + g1c * mc
            h2c = rms_norm(ctx, norm2_g[layer]) * (1.0 + sc2c) + sh2c
            tokens = jnp.concatenate([tokens, h2c.reshape(-1, d)], axis=0)
        y = hier_moe(tokens, router_grp_w[layer], router_grp_b[layer], router_exp_w[layer], router_exp_b[layer],
                     moe_w_gate[layer], moe_w_up[layer], moe_w_down[layer])
        x = x + g2 * y[: b * n_lat].reshape(x.shape)
        if want_ctx:
            ctx = ctx + g2c * y[b * n_lat:].reshape(ctx.shape)
    return x
```

```python
import contextlib
import numpy as np
import concourse.bass as bass
import concourse.mybir as mybir

F32 = mybir.dt.float32
BF16 = mybir.dt.bfloat16
I32 = mybir.dt.int32
AF = mybir.ActivationFunctionType
ALU = mybir.AluOpType
AX = mybir.AxisListType

COMPUTE = ("pe", "act", "dve", "pool")
ENGATTR = {"pe": "tensor", "act": "scalar", "dve": "vector", "pool": "gpsimd", "sp": "sync"}


class V:
    __slots__ = ("ap", "key")

    def __init__(self, ap, key):
        self.ap = ap
        self.key = key

    def __getitem__(self, idx):
        return V(self.ap[idx], self.key)

    def __getattr__(self, name):
        a = getattr(self.ap, name)
        if callable(a):
            def f(*args, **kw):
                r = a(*args, **kw)
                return V(r, self.key) if isinstance(r, type(self.ap)) else r
            return f
        return a


def _key(x):
    return x.key if isinstance(x, V) else x.tensor.name


def _flat(keys):
    out = []
    for k in keys:
        if isinstance(k, tuple):
            out.extend(k)
        else:
            out.append(k)
    return out


def _ap(x):
    return x.ap if isinstance(x, V) else x


def _isnum(x):
    return isinstance(x, (int, float))


class Prog:
    def __init__(self, nc, arena_bytes=212480):
        self.nc = nc
        self.ops = []
        self.eng_ops = {e: [] for e in ENGATTR}
        self.state = {}
        self.es = contextlib.ExitStack()
        self.last = {}
        self.pending_dma = []
        self.arena = self.es.enter_context(nc.sbuf_tensor("arena", [128, arena_bytes // 4], F32))
        self.arena_bytes = arena_bytes
        self.aptr = 0
        self.nalloc = 0
        self.psum = self.es.enter_context(nc.psum_tensor("psum_all", [128, 4096], F32))

    def sb(self, name, shape, dtype, parts=128):
        esz = mybir.dt.size(dtype)
        n = 1
        for s in shape[1:]:
            n *= s
        nbytes = (n * esz + 63) // 64 * 64
        assert self.aptr + nbytes <= self.arena_bytes, (name, self.aptr, nbytes)
        a = self.arena[0:shape[0], self.aptr // 4:(self.aptr + nbytes) // 4]
        if dtype != F32:
            a = a.bitcast(dtype)
        if a.shape[-1] != n:
            a = a[:, 0:n]
        self.aptr += nbytes
        self.nalloc += 1
        v = V(a, "%s#%d" % (name, self.nalloc))
        if len(shape) > 2:
            names = " ".join("d%d" % i for i in range(1, len(shape)))
            v = v.rearrange("p (%s) -> p %s" % (names, names),
                            **{"d%d" % i: shape[i] for i in range(1, len(shape))})
        return v

    def mark(self):
        return self.aptr

    def release(self, mark):
        self.aptr = mark

    def bank(self, i, dtype=F32, n=1):
        a = self.psum[:, 512 * i:512 * (i + n)]
        if dtype != F32:
            a = a.bitcast(dtype)
        if n == 1:
            return V(a, "psb%d" % i)
        return V(a, tuple("psb%d" % j for j in range(i, i + n)))

    def dram(self, name, shape, dtype, kind="Internal"):
        return self.nc.dram_tensor(name, list(shape), dtype, kind=kind).ap()

    def _collapse(self, deps):
        best = {}
        out = set()
        for d in deps:
            o = self.ops[d]
            if o["dma"]:
                out.add(d)
            else:
                b = best.get(o["eng"])
                if b is None or d > b:
                    best[o["eng"]] = d
        out.update(best.values())
        return out

    def op(self, eng, fn, reads=(), writes=(), dma=False):
        idx = len(self.ops)
        reads = list(dict.fromkeys(_flat(reads)))
        writes = list(dict.fromkeys(_flat(writes)))
        o = dict(eng=eng, fn=fn, dma=dma, dkey=(writes[0] if dma else None))
        self.ops.append(o)
        raw = set()
        other = set()
        for k in reads:
            st = self.state.setdefault(k, dict(w=[], r=[]))
            raw.update(st["w"])
        for k in writes:
            st = self.state.setdefault(k, dict(w=[], r=[]))
            if k in reads or st["r"]:
                other.update(st["w"])
                other.update(st["r"])
                st["pd"] = set(st["w"]) | set(st["r"])
                st["w"] = [idx]
                st["r"] = []
            else:
                other.update(st.get("pd", ()))
                st["w"].append(idx)
                if len(st["w"]) > 48:
                    st["w"] = list(self._collapse(st["w"]))
        for k in reads:
            if k not in writes:
                st = self.state[k]
                st["r"].append(idx)
                if len(st["r"]) > 48:
                    st["r"] = list(self._collapse(st["r"]))
        if not dma:
            other = {d for d in other if self.ops[d]["dma"] or self.ops[d]["eng"] != eng}
        deps = raw | other
        deps.discard(idx)
        o["deps"] = self._collapse(deps)
        self.eng_ops[eng].append(idx)
        if dma:
            self.pending_dma.append(idx)
        else:
            self.last[eng] = idx
        return idx

    def barrier(self):
        deps = set(self.last.values()) | set(self.pending_dma)
        self.nbar = getattr(self, "nbar", 0) + 1
        for e in ENGATTR:
            idx = len(self.ops)
            o = dict(eng=e, fn=None, dma=False, dkey=None, deps=set(deps), bar=self.nbar)
            self.ops.append(o)
            self.eng_ops[e].append(idx)
        self.pending_dma = []
        self.state = {}

    def dma(self, eng, out, in_, **kw):
        o_, i_ = _ap(out), _ap(in_)
        return self.op(eng, lambda e: e.dma_start(out=o_, in_=i_, **kw),
                       [_key(in_)], [_key(out)], dma=True)

    def mm(self, out, lhsT, rhs, start=True, stop=True, **kw):
        o_, l_, r_ = _ap(out), _ap(lhsT), _ap(rhs)
        return self.op("pe", lambda e: e.matmul(o_, l_, r_, start=start, stop=stop, **kw),
                       [_key(lhsT), _key(rhs)], [_key(out)])

    def tr(self, out, in_, ident):
        o_, i_, d_ = _ap(out), _ap(in_), _ap(ident)
        return self.op("pe", lambda e: e.transpose(o_, i_, d_),
                       [_key(in_), _key(ident)], [_key(out)])

    def act(self, out, in_, func, bias=None, scale=1.0, accum_out=None, eng="act"):
        rd = [_key(in_)]
        kw = {}
        if bias is not None:
            kw["bias"] = _ap(bias)
            if not _isnum(bias):
                rd.append(_key(bias))
        if not _isnum(scale):
            rd.append(_key(scale))
        kw["scale"] = _ap(scale)
        wr = [_key(out)]
        if accum_out is not None:
            kw["accum_out"] = _ap(accum_out)
            wr.append(_key(accum_out))
        o_, i_ = _ap(out), _ap(in_)
        return self.op(eng, lambda e: e.activation(o_, i_, func, **kw), rd, wr)

    def tt(self, out, in0, in1, op, eng="dve"):
        o_, a_, b_ = _ap(out), _ap(in0), _ap(in1)
        return self.op(eng, lambda e: e.tensor_tensor(o_, a_, b_, op),
                       [_key(in0), _key(in1)], [_key(out)])

    def ts(self, out, in0, s1, s2, op0, op1=None, accum_out=None, eng="dve"):
        rd = [_key(in0)]
        for s in (s1, s2):
            if s is not None and not _isnum(s):
                rd.append(_key(s))
        wr = [_key(out)]
        kw = {}
        if accum_out is not None:
            kw["accum_out"] = _ap(accum_out)
            wr.append(_key(accum_out))
        o_, a_, s1_, s2_ = _ap(out), _ap(in0), _ap(s1), _ap(s2)
        if op1 is None:
            return self.op(eng, lambda e: e.tensor_scalar(o_, a_, s1_, None, op0, **kw), rd, wr)
        return self.op(eng, lambda e: e.tensor_scalar(o_, a_, s1_, s2_, op0, op1, **kw), rd, wr)

    def stt(self, out, in0, scalar, in1, op0, op1, eng="dve"):
        rd = [_key(in0), _key(in1)]
        if not _isnum(scalar):
            rd.append(_key(scalar))
        o_, a_, s_, b_ = _ap(out), _ap(in0), _ap(scalar), _ap(in1)
        return self.op(eng, lambda e: e.scalar_tensor_tensor(o_, a_, s_, b_, op0, op1),
                       rd, [_key(out)])

    def copy(self, out, in_, eng="dve"):
        o_, i_ = _ap(out), _ap(in_)
        if eng == "act":
            return self.op(eng, lambda e: e.copy(o_, i_), [_key(in_)], [_key(out)])
        return self.op(eng, lambda e: e.tensor_copy(o_, i_), [_key(in_)], [_key(out)])

    def memset(self, out, val, eng="dve"):
        o_ = _ap(out)
        return self.op(eng, lambda e: e.memset(o_, val), [], [_key(out)])

    def reduce(self, out, in_, op, axis=AX.X, eng="dve"):
        o_, i_ = _ap(out), _ap(in_)
        return self.op(eng, lambda e: e.tensor_reduce(o_, i_, axis, op), [_key(in_)], [_key(out)])

    def recip(self, out, in_):
        o_, i_ = _ap(out), _ap(in_)
        return self.op("dve", lambda e: e.reciprocal(o_, i_), [_key(in_)], [_key(out)])

    def generic(self, eng, fn, reads, writes):
        return self.op(eng, fn, [_key(a) for a in reads], [_key(a) for a in writes])

    def emit(self, final_keys=()):
        nc = self.nc
        ops = self.ops

        def skip(o, po):
            return (not po["dma"]) and (not o["dma"]) and po["eng"] == "pe" and o["eng"] == "pe" \
                and not o.get("bar")

        observed = set()
        for o in ops:
            for d in o["deps"]:
                po = ops[d]
                if po["dma"] or skip(o, po):
                    continue
                observed.add(d)
        finals = []
        for k in final_keys:
            st = self.state.get(k)
            if st:
                for d in st["w"]:
                    finals.append(d)
                    if not ops[d]["dma"]:
                        observed.add(d)
        sems = {e: self.es.enter_context(nc.semaphore("s_" + e)) for e in COMPUTE}
        pool = []
        pcnt = []
        dsem = {}
        cnt = {e: 0 for e in COMPUTE}
        semval = {}
        epoch = 0
        for i, o in enumerate(ops):
            if o.get("bar") and o["bar"] != epoch:
                epoch = o["bar"]
                dsem = {}
            if o["dma"]:
                g = o["dkey"]
                if g not in dsem:
                    j = len(dsem)
                    if j >= len(pool):
                        pool.append(self.es.enter_context(nc.semaphore("d%d" % j)))
                        pcnt.append(0)
                    dsem[g] = j
                j = dsem[g]
                pcnt[j] += 16
                semval[i] = (pool[j], pcnt[j])
            elif i in observed:
                cnt[o["eng"]] += 1
                semval[i] = (sems[o["eng"]], cnt[o["eng"]])
        self.n_sems = 4 + len(pool)
        self.sem_max = dict(cnt)
        with nc.Block() as block:
            for ename, attr in ENGATTR.items():
                lst = self.eng_ops[ename]
                extra = finals if ename == "sp" else []
                if not lst and not extra:
                    continue

                def section(eng, ename=ename, lst=lst, extra=extra):
                    waited = {}

                    def do_waits(deps, o):
                        need = {}
                        for d in deps:
                            po = ops[d]
                            if o is not None and skip(o, po):
                                continue
                            if d not in semval:
                                continue
                            s, v = semval[d]
                            if waited.get(s.num, 0) >= v:
                                continue
                            if need.get(s.num, (None, 0))[1] < v:
                                need[s.num] = (s, v)
                        for s, v in need.values():
                            waited[s.num] = v
                            eng.wait_ge(s, v)

                    for i in lst:
                        o = ops[i]
                        do_waits(o["deps"], o)
                        if o.get("bar"):
                            continue
                        ins = o["fn"](eng)
                        if i in semval:
                            ins.then_inc(semval[i][0], 16 if o["dma"] else 1)
                    do_waits(extra, None)

                getattr(block, attr)(section)
        self.es.close()


D = 1024
IN_COLS = 7680
EPS = 1e-6


class Cfg:
    def __init__(self, NL=4096, NC=256, layers=(0, 1, 2, 3), nlayers_total=4, want_ctx_last=False):
        self.NL, self.NC = NL, NC
        self.N = NL + NC
        self.NT = self.N // 128
        self.NTC = NC // 128
        self.layers = list(layers)
        self.LT = nlayers_total
        self.want_ctx_last = want_ctx_last


WEIGHT_SPECS = [
    ("ada_w", (D, 6 * D)), ("ada_b", (6 * D,)), ("norm1_g", (D,)), ("norm2_g", (D,)),
    ("w_in", (D, IN_COLS)), ("att_qn_g", (64,)), ("att_kn_g", (64,)), ("ret_decay_logit", (2, 4)),
    ("ret_gn_g", (512,)), ("rwkv_mu", (2, 1792)), ("rwkv_w0", (2, 512)), ("rwkv_w2", (2, 64, 512)),
    ("rwkv_a0", (2, 512)), ("rwkv_a2", (2, 64, 512)), ("rwkv_g2", (128, 512)), ("rwkv_k_k", (512,)),
    ("rwkv_k_a", (512,)), ("rwkv_r_k", (8, 64)), ("rwkv_ln_g", (512,)), ("rwkv_ln_b", (512,)),
    ("w_branch", (3, 512, D)), ("w_out", (D, D)), ("router_grp_w", (D, 4)), ("router_grp_b", (4,)),
    ("router_exp_w", (D, 32)), ("router_exp_b", (32,)), ("moe_w_gate", (32, D, 512)),
    ("moe_w_up", (32, D, 512)), ("moe_w_down", (32, 512, D)),
]


def host_consts(cfg):
    c = {}
    c["ident"] = np.eye(128, dtype=np.float32)
    for nm, hd in (("att", 64), ("ret", 128)):
        rows = cfg.NL // 64
        row = np.broadcast_to(np.arange(rows, dtype=np.float32)[:, None], (rows, 64)).reshape(-1)
        col = np.broadcast_to(np.arange(64, dtype=np.float32)[None, :], (rows, 64)).reshape(-1)
        quarter = hd // 4
        inv = (np.float32(10000.0) ** (-np.arange(quarter, dtype=np.float32) / np.float32(quarter))).astype(np.float32)
        ang = np.stack([row[:, None] * inv, col[:, None] * inv], axis=1).astype(np.float32)
        c[nm + "_cos"] = np.cos(ang).reshape(cfg.NL, 2 * quarter).astype(np.float32)
        c[nm + "_sin"] = np.sin(ang).reshape(cfg.NL, 2 * quarter).astype(np.float32)
    sidx = np.arange(128, dtype=np.float32)[:, None]
    tidx = np.arange(128, dtype=np.float32)[None, :]
    lag = tidx - sidx
    ones = np.ones((128, 128), np.float32)
    c["retc"] = np.concatenate([np.maximum(lag, 0), np.maximum(-lag, 0), (lag >= 0).astype(np.float32),
                                (lag <= 0).astype(np.float32), ones * (tidx + 1), ones * (128 - tidx)], 1).astype(np.float32)
    le = (lag >= 0).astype(np.float32); ge = (lag <= 0).astype(np.float32)
    lt = (lag > 0).astype(np.float32); gt = (lag < 0).astype(np.float32)
    c["rwmask"] = np.concatenate([le, ge, ones, lt, le, gt, ge], 1).astype(np.float32)
    c["retcol"] = np.concatenate([sidx, 127 - sidx], 1).astype(np.float32)
    return c


class Ctx:
    pass


def cast_load(P, dst, src, eng="pool"):
    n = src.shape[-1]
    step = 2048
    for a in range(0, n, step):
        b = min(n, a + step)
        P.dma(eng, dst[..., a:b], src[..., a:b])


def phase0(P, cfg, T):
    P.dma("sp", T.XS[0:cfg.NC, :], T.ctx)
    P.dma("sp", T.XS[cfg.NC:cfg.N, :], T.x)
    P.barrier()


def phase_ada(P, cfg, T, l, li):
    m0 = P.mark()
    craw = P.sb("craw", [128, 2, 8], F32)
    cT = P.sb("cT", [128, 8, 2], F32)
    P.dma("sp", craw[:, 0, :], T.c.rearrange("o (kt p) -> p (o kt)", p=128), allow_slow_non_contiguous=True)
    P.dma("sp", craw[:, 1, :], T.c_ctx.rearrange("o (kt p) -> p (o kt)", p=128), allow_slow_non_contiguous=True)
    P.act(cT.rearrange("p k t -> p t k"), craw, AF.Silu)
    mod = P.sb("mod", [2, 6 * D], F32)
    gn = P.sb("gn", [2, 2, D], F32)
    P.dma("sp", mod, T.ada_b[li:li + 1, :].to_broadcast([2, 6 * D]))
    P.dma("sp", gn[:, 0, :], T.norm1_g[li:li + 1, :].to_broadcast([2, D]))
    P.dma("sp", gn[:, 1, :], T.norm2_g[li:li + 1, :].to_broadcast([2, D]))
    wsl = [P.sb("adaw%d" % s, [128, 8, 512], F32) for s in range(2)]
    wv = T.ada_w[li].rearrange("(kt p) c -> p kt c", p=128)
    for cc in range(12):
        w = wsl[cc % 2]
        P.dma("sp" if cc % 2 == 0 else "act", w, wv[:, :, cc * 512:(cc + 1) * 512])
        ps = P.bank(cc % 2)
        for kt in range(8):
            P.mm(ps[0:2, :], cT[:, kt, :], w[:, kt, :], start=(kt == 0), stop=(kt == 7))
        P.tt(mod[:, cc * 512:(cc + 1) * 512], mod[:, cc * 512:(cc + 1) * 512], ps[0:2, :], ALU.add)
    for slot, g in ((1, 0), (4, 1)):
        P.stt(mod[:, slot * D:(slot + 1) * D], mod[:, slot * D:(slot + 1) * D], 1.0, gn[:, g, :],
              ALU.add, ALU.mult)
    P.dma("sp", T.MOD[l], mod)
    P.barrier()
    P.release(m0)


def load_mod(P, T, l, slot, name):
    out = []
    for r in (1, 0):
        t = P.sb("%s_%d" % (name, r), [128, D], F32)
        P.dma("sp", t, T.MOD[l][r:r + 1, slot * D:(slot + 1) * D].to_broadcast([128, D]))
        out.append(t)
    return out


def rsqrt_to(P, out, in_, scale, eps):
    P.act(out, in_, AF.Sqrt, bias=eps, scale=scale)
    P.recip(out, out)


def rms_mod_tile(P, x, G, sh, hf, h, ss, rstd):
    P.act(hf, x, AF.Square, accum_out=ss)
    rsqrt_to(P, rstd, ss, 1.0 / D, EPS)
    P.stt(hf, x, rstd, G, ALU.mult, ALU.mult)
    P.tt(h, hf, sh, ALU.add)


def phase_inproj(P, cfg, T, l, li):
    m0 = P.mark()
    Wb = P.sb("Wb", [128, 8, IN_COLS], BF16)
    wv = T.w_in[li].rearrange("(kt p) c -> p kt c", p=128)
    for kt in range(8):
        cast_load(P, Wb[:, kt, :], wv[:, kt, :])
    G = load_mod(P, T, l, 1, "G1")
    SH = load_mod(P, T, l, 0, "SH1")
    xs = [P.sb("xa%d" % s, [128, D], F32) for s in range(2)]
    hf = P.sb("hf", [128, D], F32)
    h = P.sb("h", [128, D], BF16)
    hT = P.sb("hT", [128, 8, 128], BF16)
    ss = P.sb("ss", [128, 1], F32)
    rstd = P.sb("rstd", [128, 1], F32)
    ust = [P.sb("ust%d" % s, [128, IN_COLS], BF16) for s in range(2)]
    pT = P.bank(0, BF16)
    nb = 0
    for i in range(cfg.NT):
        x = xs[i % 2]
        isl = 0 if i < cfg.NTC else 1
        P.dma("sp", x, T.XS[i * 128:(i + 1) * 128, :])
        rms_mod_tile(P, x, G[isl], SH[isl], hf, h, ss, rstd)
        for kt in range(8):
            P.tr(pT[:, kt * 128:(kt + 1) * 128], h[:, kt * 128:(kt + 1) * 128], T.identb)
        P.copy(hT.rearrange("p k t -> p (k t)"), pT[:, 0:1024], eng="act")
        u = ust[i % 2]
        for cc in range(IN_COLS // 512):
            ps = P.bank(1 + nb % 7)
            nb += 1
            for kt in range(8):
                P.mm(ps, hT[:, kt, :], Wb[:, kt, cc * 512:(cc + 1) * 512], start=(kt == 0), stop=(kt == 7))
            P.copy(u[:, cc * 512:(cc + 1) * 512], ps, eng=("act" if cc % 2 else "dve"))
        P.dma("sp", T.U[i * 128:(i + 1) * 128, :], u)
    P.barrier()
    P.release(m0)


def rope_apply(P, dst, src, cos, sin, tmp, nh, q):
    v = lambda a: a.rearrange("p (h a t q) -> p h a t q", h=nh, a=2, t=2, q=q)
    s5, d5 = v(src), v(dst)
    x1, x2 = s5[:, :, :, 0, :], s5[:, :, :, 1, :]
    c4 = cos.rearrange("p (a q) -> p a q", a=2).unsqueeze(1).to_broadcast([128, nh, 2, q])
    s4 = sin.rearrange("p (a q) -> p a q", a=2).unsqueeze(1).to_broadcast([128, nh, 2, q])
    t1 = tmp[0].rearrange("p (h a q) -> p h a q", h=nh, a=2, q=q)
    t2 = tmp[1].rearrange("p (h a q) -> p h a q", h=nh, a=2, q=q)
    P.tt(t1, x1, c4, ALU.mult)
    P.tt(t2, x2, s4, ALU.mult)
    P.tt(d5[:, :, :, 0, :], t1, t2, ALU.subtract)
    P.tt(t1, x2, c4, ALU.mult)
    P.tt(t2, x1, s4, ALU.mult)
    P.tt(d5[:, :, :, 1, :], t1, t2, ALU.add)


def phase_attn(P, cfg, T, l, li, want_ctx):
    m0 = P.mark()
    NT, NTC, N = cfg.NT, cfg.NTC, cfg.N
    QT = P.sb("QT", [64, NT, 8, 128], BF16)
    KT = P.sb("KT", [64, 2, N], BF16)
    Vg = P.sb("Vg", [128, NT, 2, 65], BF16)
    gains = P.sb("gains", [128, 10, 64], F32)
    P.dma("sp", gains[:, 0, :], T.att_qn_g[li:li + 1, :].to_broadcast([128, 64]))
    P.dma("sp", gains[:, 8, :], T.att_kn_g[li:li + 1, :].to_broadcast([128, 64]))
    for hh in range(1, 8):
        P.copy(gains[:, hh, :], gains[:, 0, :], eng="pool")
    P.copy(gains[:, 9, :], gains[:, 8, :], eng="pool")
    P.memset(Vg[:, :, :, 64:65], 1.0, eng="pool")
    onesr = P.sb("onesr", [65, 64], F32)
    P.memset(onesr, 1.0, eng="pool")
    qkv = [P.sb("qkv%d" % s, [128, 768], BF16) for s in range(2)]
    cs = [P.sb("cs%d" % s, [128, 2, 32], F32) for s in range(2)]
    sq = P.sb("sq", [128, 640], F32)
    ssq = P.sb("ssq", [128, 10], F32)
    qn = P.sb("qn", [128, 640], F32)
    qr = P.sb("qr", [128, 640], BF16)
    tmp = [P.sb("rt%d" % s, [128, 320], F32) for s in range(2)]
    pTa = P.bank(6, BF16)
    pTb = P.bank(7, BF16)
    for i in range(NT):
        t = qkv[i % 2]
        P.dma("sp", t, T.U[i * 128:(i + 1) * 128, 0:768])
        P.tt(sq, t[:, 0:640], t[:, 0:640], ALU.mult)
        P.reduce(ssq, sq.rearrange("p (h d) -> p h d", h=10), ALU.add)
        rsqrt_to(P, ssq, ssq, 1.0 / 64, EPS)
        qn3 = qn.rearrange("p (h d) -> p h d", h=10)
        P.tt(qn3, t[:, 0:640].rearrange("p (h d) -> p h d", h=10),
             ssq.unsqueeze(2).to_broadcast([128, 10, 64]), ALU.mult)
        if i >= NTC:
            c = cs[i % 2]
            t0 = (i - NTC) * 128
            P.dma("act", c[:, 0, :], T.c_att_cos[t0:t0 + 128, :])
            P.dma("act", c[:, 1, :], T.c_att_sin[t0:t0 + 128, :])
            P.tt(qn3, qn3, gains, ALU.mult)
            rope_apply(P, qr, qn, c[:, 0, :], c[:, 1, :], tmp, 10, 16)
        else:
            P.tt(qr.rearrange("p (h d) -> p h d", h=10), qn3, gains, ALU.mult)
        for hh in range(8):
            P.tr(pTa[0:64, hh * 128:(hh + 1) * 128], qr[:, hh * 64:(hh + 1) * 64], T.identb)
        for kv in range(2):
            P.tr(pTb[0:64, kv * 128:(kv + 1) * 128], qr[:, 512 + kv * 64:512 + (kv + 1) * 64], T.identb)
        P.copy(QT[:, i, :, :].rearrange("p h t -> p (h t)"), pTa[0:64, 0:1024], eng="act")
        P.copy(KT[:, :, i * 128:(i + 1) * 128], pTb[0:64, 0:256].rearrange("p (k t) -> p k t", k=2), eng="act")
        P.copy(Vg[:, i, :, 0:64], t[:, 640:768].rearrange("p (k d) -> p k d", k=2), eng="pool")
    pts = [P.sb("pt%d" % s, [128, 1024], BF16) for s in range(2)]
    rsb = P.sb("rsb", [65, 512], F32)
    bcs = P.sb("bcs", [64, 512], F32)
    yts = [P.sb("yt%d" % s, [64, 512], BF16) for s in range(2)]
    it = 0
    grp = 0
    qtiles = list(range(NTC, NT)) + (list(range(NTC)) if want_ctx else [])
    for qi in qtiles:
        nk = NT if qi >= NTC else NTC
        for g in range(2):
            ob = P.bank(4 + it % 2)
            rhsq = QT[:, qi, 4 * g:4 * g + 4, :].rearrange("p h t -> p (h t)")
            for j0 in range(0, nk, 2):
                sb2 = P.bank(2 * (grp % 2), n=2)
                pt = pts[grp % 2]
                grp += 1
                nj = min(2, nk - j0)
                for jj in range(nj):
                    j = j0 + jj
                    P.mm(sb2[:, jj * 512:(jj + 1) * 512], KT[:, g, j * 128:(j + 1) * 128], rhsq)
                P.act(pt[:, 0:512 * nj], sb2[:, 0:512 * nj], AF.Exp, scale=0.125)
                for jj in range(nj):
                    j = j0 + jj
                    P.mm(ob[0:65, :], Vg[:, j, g, :], pt[:, jj * 512:(jj + 1) * 512],
                         start=(j == 0), stop=(j == nk - 1))
            P.recip(rsb[64:65, :], ob[64:65, :])
            bc = P.bank(6)
            P.mm(bc[0:64, :], onesr[64:65, :], rsb[64:65, :])
            P.copy(bcs, bc[0:64, :], eng="act")
            yt = yts[it % 2]
            P.tt(yt, ob[0:64, :], bcs, ALU.mult)
            P.dma("sp", T.YAT[4 * g:4 * g + 4, :, qi * 128:(qi + 1) * 128].rearrange("h d t -> d h t"),
                  yt.rearrange("p (h t) -> p h t", h=4))
            it += 1
    P.barrier()
    P.release(m0)


def phase_ret(P, cfg, T, l, li, want_ctx):
    m0 = P.mark()
    NT, NTC, N = cfg.NT, cfg.NTC, cfg.N
    SC = 128.0 ** -0.5
    rc = P.sb("retc", [128, 6, 128], F32)
    P.dma("sp", rc, T.c_retc.rearrange("p (a t) -> p a t", a=6))
    cols = P.sb("retcol", [128, 2], F32)
    P.dma("sp", cols, T.c_retcol)
    lg = P.sb("lg", [128, 8], F32)
    P.dma("sp", lg, T.ret_decay_logit[li:li + 1].rearrange("o a h -> o (a h)").to_broadcast([128, 8]))
    P.act(lg, lg, AF.Exp, scale=-1.0)
    P.act(lg, lg, AF.Ln, bias=1.0)
    P.ts(lg, lg, -1.0, None, ALU.mult)
    DT = P.sb("DT", [128, 4, 128], F32)
    DQ = P.sb("DQ", [128, 2, 4, 128], F32)
    e1 = P.sb("e1", [128, 128], F32)
    for h in range(4):
        P.act(e1, rc[:, 0, :], AF.Exp, scale=lg[:, h:h + 1])
        P.tt(DT[:, h, :], e1, rc[:, 2, :], ALU.mult)
        P.act(e1, rc[:, 1, :], AF.Exp, scale=lg[:, 4 + h:5 + h])
        P.tt(e1, e1, rc[:, 3, :], ALU.mult)
        P.stt(DT[:, h, :], DT[:, h, :], SC, e1, ALU.mult, ALU.mult) if False else None
        P.tt(DT[:, h, :], DT[:, h, :], e1, ALU.add)
        P.act(DQ[:, 0, h, :], rc[:, 4, :], AF.Exp, scale=lg[:, h:h + 1])
        P.act(DQ[:, 1, h, :], rc[:, 5, :], AF.Exp, scale=lg[:, 4 + h:5 + h])
    P.ts(DT, DT, SC, None, ALU.mult)
    dk = P.sb("dk", [128, 2, 4], F32)
    for h in range(4):
        P.act(dk[:, 0, h:h + 1], cols[:, 1:2], AF.Exp, scale=lg[:, h:h + 1])
        P.act(dk[:, 1, h:h + 1], cols[:, 0:1], AF.Exp, scale=lg[:, 4 + h:5 + h])
    P.ts(dk, dk, SC, None, ALU.mult)
    gC = P.sb("gC", [128, 8], F32)
    P.act(gC, lg, AF.Exp, scale=128.0)
    gng = P.sb("gng", [128, 512], F32)
    P.dma("sp", gng, T.ret_gn_g[li:li + 1, :].to_broadcast([128, 512]))

    import os
    stop = int(os.environ.get("RET_STOP", "9"))
    if stop <= 1:
        P.barrier(); P.release(m0); return
    Krot = P.sb("Krot", [128, NT, 512], BF16)
    Vs = P.sb("Vs", [128, NT, 512], BF16)
    Sf = P.sb("Sf", [128, NT + 1, 512], BF16)
    Sm = P.sb("Sm", [128, 512], F32)
    Sbb = P.sb("Sbb", [128, 512], BF16)
    kin = [P.sb("kin%d" % s, [128, 512], BF16) for s in range(2)]
    cs = [P.sb("rcs%d" % s, [128, 2, 64], F32) for s in range(2)]
    tmp = [P.sb("rrt%d" % s, [128, 256], F32) for s in range(2)]
    Kd = P.sb("Kd", [128, 512], BF16)
    P.memset(Sm, 0.0)
    P.memset(Sf[:, 0, :], 0.0)
    for c in range(NT):
        k = kin[c % 2]
        P.dma("sp", k, T.U[c * 128:(c + 1) * 128, 1280:1792])
        P.dma("act", Vs[:, c, :], T.U[c * 128:(c + 1) * 128, 1792:2304])
        if c >= NTC:
            cc = cs[c % 2]
            t0 = (c - NTC) * 128
            P.dma("sp", cc[:, 0, :], T.c_ret_cos[t0:t0 + 128, :])
            P.dma("sp", cc[:, 1, :], T.c_ret_sin[t0:t0 + 128, :])
            rope_apply(P, Krot[:, c, :], k, cc[:, 0, :], cc[:, 1, :], tmp, 4, 32)
        else:
            P.copy(Krot[:, c, :], k)
        P.tt(Kd.rearrange("p (h d) -> p h d", h=4), Krot[:, c, :].rearrange("p (h d) -> p h d", h=4),
             dk[:, 0, :].unsqueeze(2).to_broadcast([128, 4, 128]), ALU.mult)
        ps = P.bank(c % 2)
        for h in range(4):
            P.mm(ps[:, h * 128:(h + 1) * 128], Kd[:, h * 128:(h + 1) * 128], Vs[:, c, h * 128:(h + 1) * 128])
        Sm3 = Sm.rearrange("p (h d) -> p h d", h=4)
        P.tt(Sm3, Sm3, gC[:, 0:4].unsqueeze(2).to_broadcast([128, 4, 128]), ALU.mult)
        P.tt(Sm, Sm, ps, ALU.add)
        P.copy(Sf[:, c + 1, :], Sm, eng="act")
    if stop <= 2:
        P.barrier(); P.release(m0); return
    P.memset(Sm, 0.0)
    P.memset(Sbb, 0.0)
    qg = [P.sb("qg%d" % s, [128, 2, 512], BF16) for s in range(2)]
    Qr = P.sb("Qr", [128, 512], BF16)
    QTs = P.sb("QTs", [128, 3, 512], BF16)
    KTs = P.sb("KTs", [128, 512], BF16)
    AT = P.sb("AT", [128, 512], BF16)
    yf = P.sb("yf", [128, 512], F32)
    ysq = P.sb("ysq", [128, 512], F32)
    rs = P.sb("rrs", [128, 4], F32)
    sg = P.sb("sg", [128, 512], F32)
    yo = P.sb("yo", [128, 512], BF16)
    yT = [P.sb("yT%d" % s, [128, 4, 128], BF16) for s in range(2)]
    order = list(range(NTC - 1, -1, -1)) + list(range(NT - 1, NTC - 1, -1))
    for n_, c in enumerate(order):
        need_out = want_ctx or c >= NTC
        if need_out:
            t = qg[n_ % 2]
            P.dma("sp", t[:, 0, :], T.U[c * 128:(c + 1) * 128, 768:1280])
            P.dma("act", t[:, 1, :], T.U[c * 128:(c + 1) * 128, 2304:2816])
            if c >= NTC:
                cc = cs[n_ % 2]
                t0 = (c - NTC) * 128
                P.dma("sp", cc[:, 0, :], T.c_ret_cos[t0:t0 + 128, :])
                P.dma("sp", cc[:, 1, :], T.c_ret_sin[t0:t0 + 128, :])
                rope_apply(P, Qr, t[:, 0, :], cc[:, 0, :], cc[:, 1, :], tmp, 4, 32)
                qsrc = Qr
            else:
                qsrc = t[:, 0, :]
            pq = P.bank(2, BF16)
            pk = P.bank(3, BF16)
            for h in range(4):
                P.tr(pq[:, h * 128:(h + 1) * 128], qsrc[:, h * 128:(h + 1) * 128], T.identb)
                P.tr(pk[:, h * 128:(h + 1) * 128], Krot[:, c, h * 128:(h + 1) * 128], T.identb)
            P.copy(QTs[:, 0, :], pq[:, 0:512], eng="act")
            P.tt(QTs[:, 1, :], QTs[:, 0, :], DQ[:, 0, :, :].rearrange("p h t -> p (h t)"), ALU.mult)
            P.tt(QTs[:, 2, :], QTs[:, 0, :], DQ[:, 1, :, :].rearrange("p h t -> p (h t)"), ALU.mult)
            P.copy(KTs, pk[:, 0:512], eng="act")
            if stop <= 3:
                continue
            psc = P.bank(4)
            for h in range(4):
                P.mm(psc[:, h * 128:(h + 1) * 128], KTs[:, h * 128:(h + 1) * 128], QTs[:, 0, h * 128:(h + 1) * 128])
            P.tt(AT, psc, DT.rearrange("p h t -> p (h t)"), ALU.mult)
            py = P.bank(5)
            for h in range(4):
                hs = slice(h * 128, (h + 1) * 128)
                P.mm(py[:, hs], AT[:, hs], Vs[:, c, hs], start=True, stop=False)
                P.mm(py[:, hs], QTs[:, 1, hs], Sf[:, c, hs], start=False, stop=False)
                P.mm(py[:, hs], QTs[:, 2, hs], Sbb[:, hs], start=False, stop=True)
            if stop <= 4:
                continue
            P.copy(yf, py, eng="act")
            P.tt(ysq, yf, yf, ALU.mult)
            P.reduce(rs, ysq.rearrange("p (h d) -> p h d", h=4), ALU.add)
            rsqrt_to(P, rs, rs, 1.0 / 128, EPS)
            P.tt(yf.rearrange("p (h d) -> p h d", h=4), yf.rearrange("p (h d) -> p h d", h=4),
                 rs.unsqueeze(2).to_broadcast([128, 4, 128]), ALU.mult)
            P.tt(yf, yf, gng, ALU.mult)
            P.act(sg, t[:, 1, :], AF.Silu)
            P.tt(yo, yf, sg, ALU.mult)
            if stop <= 5:
                continue
            pt = P.bank(6, BF16)
            for a in range(4):
                P.tr(pt[:, a * 128:(a + 1) * 128], yo[:, a * 128:(a + 1) * 128], T.identb)
            yt = yT[n_ % 2]
            P.copy(yt.rearrange("p a t -> p (a t)"), pt[:, 0:512], eng="act")
            P.dma("sp", T.YRT[:, :, c * 128:(c + 1) * 128].rearrange("a p t -> p a t"), yt)
        P.tt(Kd.rearrange("p (h d) -> p h d", h=4), Krot[:, c, :].rearrange("p (h d) -> p h d", h=4),
             dk[:, 1, :].unsqueeze(2).to_broadcast([128, 4, 128]), ALU.mult)
        ps = P.bank(n_ % 2)
        for h in range(4):
            P.mm(ps[:, h * 128:(h + 1) * 128], Kd[:, h * 128:(h + 1) * 128], Vs[:, c, h * 128:(h + 1) * 128])
        Sm3 = Sm.rearrange("p (h d) -> p h d", h=4)
        P.tt(Sm3, Sm3, gC[:, 4:8].unsqueeze(2).to_broadcast([128, 4, 128]), ALU.mult)
        P.tt(Sm, Sm, ps, ALU.add)
        P.copy(Sbb, Sm, eng="act")
    P.barrier()
    P.release(m0)


WDEC = 0.6065306597126334


def phase_rwkv(P, cfg, T, l, li, want_ctx):
    NT, NTC, N = cfg.NT, cfg.NTC, cfg.N
    m0 = P.mark()
    bc = lambda ap, n: ap.to_broadcast([128, n])
    mu = P.sb("mu", [128, 3, 1792], F32)
    P.dma("sp", mu[:, 1, :], bc(T.rwkv_mu[li, 0:1, :], 1792))
    P.dma("sp", mu[:, 2, :], bc(T.rwkv_mu[li, 1:2, :], 1792))
    P.tt(mu[:, 0, :], mu[:, 1, :], mu[:, 2, :], ALU.add)
    P.ts(mu[:, 0, :], mu[:, 0, :], -1.0, 1.0, ALU.mult, ALU.add)
    cv = P.sb("cv", [128, 7, 512], F32)
    P.dma("sp", cv[:, 0, :], bc(T.rwkv_k_k[li:li + 1, :], 512))
    P.dma("sp", cv[:, 1, :], bc(T.rwkv_k_a[li:li + 1, :], 512))
    P.dma("sp", cv[:, 2, :], bc(T.rwkv_r_k[li:li + 1].rearrange("o h d -> o (h d)"), 512))
    for d in range(2):
        P.dma("sp", cv[:, 3 + d, :], bc(T.rwkv_w0[li, d:d + 1, :], 512))
        P.dma("sp", cv[:, 5 + d, :], bc(T.rwkv_a0[li, d:d + 1, :], 512))
    Wl = P.sb("Wl", [128, 2, 512], BF16)
    for d in range(2):
        P.dma("pool", Wl[0:64, d, :], T.rwkv_w2[li, d])
        P.dma("pool", Wl[64:128, d, :], T.rwkv_a2[li, d])
    G2 = P.sb("G2", [128, 512], BF16)
    P.dma("pool", G2, T.rwkv_g2[li])
    mk = P.sb("rwmk", [128, 7, 128], F32)
    P.dma("sp", mk, T.c_rwmask.rearrange("p (a t) -> p a t", a=7))
    id64 = P.sb("id64", [64, 64], F32)
    P.dma("sp", id64, T.c_ident[0:64, 0:64])
    ones1 = P.sb("ones1", [128, 1], F32)
    P.memset(ones1, 1.0)
    us = [[P.sb("u%d_%d" % (a, s_), [128, 1792], BF16) for a in range(3)] for s_ in range(2)]
    tsh = P.sb("tsh", [128, 1792], F32)
    t2 = P.sb("t2", [128, 1792], F32)
    kk = P.sb("kk", [128, 512], F32)
    tA = P.sb("tA", [128, 512], F32)
    tB = P.sb("tB", [128, 512], F32)
    s8 = P.sb("s8", [128, 8], F32)
    xwag = P.sb("xwag", [128, 256], BF16)
    lT = P.sb("lT", [128, 256], BF16)
    lw = [P.sb("lw%d" % d, [128, 512], F32) for d in range(2)]
    av = [P.sb("av%d" % d, [128, 512], F32) for d in range(2)]
    krep = [P.sb("krep%d" % d, [128, 512], F32) for d in range(2)]
    bb = P.sb("bb", [128, 512], F32)
    vb = P.sb("vb", [128, 512], BF16)
    xout = P.sb("xout", [128, 2, 512], F32)
    cums = P.sb("cums", [128, 512], F32)
    pe_ = [P.sb("pe%d" % a, [128, 512], F32) for a in range(4)]
    Z = P.sb("Z", [128, 6, 512], BF16)
    ZT = P.sb("ZT", [64, 8, 4, 128], BF16)
    NN = P.sb("NN", [128, 8, 128], F32)
    LRB = P.sb("LRB", [128, 8, 128], BF16)
    AAK = P.sb("AAK", [128, 8, 128], BF16)
    LRK = P.sb("LRK", [128, 8, 128], BF16)
    AA = P.sb("AA", [128, 8, 128], F32)
    Nk = [P.sb("Nk%d" % a, [128, 8, 128], F32) for a in range(2)]
    Ak = [P.sb("Ak%d" % a, [128, 8, 128], F32) for a in range(2)]
    MT = P.sb("MT", [128, 8, 128], F32)
    MTb = P.sb("MTb", [128, 8, 128], BF16)
    ZX = P.sb("ZX", [128, 8, 128], BF16)
    WU = P.sb("WU", [128, 8, 128], BF16)
    QE = P.sb("QE", [64, 8, 128], BF16)
    Y0s = P.sb("Y0s", [128, 512], F32)
    ptot = P.sb("ptot", [64, 512], F32)
    dg = P.sb("dg", [64, 8, 64], F32)
    GTs = P.sb("GTs", [64, 512], BF16)
    H0s = P.sb("H0s", [64, 512], BF16)
    h3 = lambda a: a.rearrange("p (h d) -> p h d", h=8)
    b8 = lambda a: a.unsqueeze(2).to_broadcast([128, 8, 64])

    for i in range(NT):
        R0 = i * 128
        uc, up, un = us[i % 2]
        first = (i == 0 or i == NTC)
        last = (i == NTC - 1 or i == NT - 1)
        P.dma("sp", uc, T.U[R0:R0 + 128, 2816:4608])
        if first:
            P.memset(up[0:1, :], 0.0, eng="pool")
            P.dma("act", up[1:128, :], T.U[R0:R0 + 127, 2816:4608])
        else:
            P.dma("act", up, T.U[R0 - 1:R0 + 127, 2816:4608])
        if last:
            P.memset(un, 0.0, eng="pool")
            P.dma("act", un[0:127, :], T.U[R0 + 1:R0 + 128, 2816:4608])
        else:
            P.dma("act", un, T.U[R0 + 1:R0 + 129, 2816:4608])
        P.tt(tsh, uc, mu[:, 0, :], ALU.mult)
        P.tt(t2, up, mu[:, 1, :], ALU.mult, eng="pool")
        P.tt(tsh, tsh, t2, ALU.add)
        P.tt(t2, un, mu[:, 2, :], ALU.mult, eng="pool")
        P.tt(tsh, tsh, t2, ALU.add)
        r_, k_, v_ = tsh[:, 0:512], tsh[:, 512:1024], tsh[:, 1024:1536]
        P.tt(kk, k_, cv[:, 0, :], ALU.mult)
        P.tt(tA, kk, kk, ALU.mult)
        P.reduce(s8, h3(tA), ALU.add)
        P.ts(s8, s8, 1e-12, None, ALU.max)
        P.act(s8, s8, AF.Sqrt)
        P.recip(s8, s8)
        P.tt(h3(kk), h3(kk), b8(s8), ALU.mult)
        P.copy(vb, v_, eng="act")
        P.copy(xwag, tsh[:, 1536:1792], eng="act")
        pl = P.bank(0, BF16)
        P.tr(pl[:, 0:128], xwag[:, 0:128], T.identb)
        P.tr(pl[:, 128:256], xwag[:, 128:256], T.identb)
        P.act(lT[0:64, 0:128], pl[0:64, 0:128], AF.Tanh)
        P.act(lT[64:128, 0:128], pl[64:128, 0:128], AF.Identity)
        P.act(lT[:, 128:256], pl[:, 128:256], AF.Sigmoid)
        for d in range(2):
            pw = P.bank(1 + d)
            P.mm(pw, lT[0:64, 0:128], Wl[0:64, d, :])
            P.tt(tA, pw, cv[:, 3 + d, :], ALU.add)
            P.act(lw[d], tA, AF.Sigmoid)
            P.ts(lw[d], lw[d], -WDEC, None, ALU.mult)
            pa = P.bank(3 + d)
            P.mm(pa, lT[64:128, 0:128], Wl[64:128, d, :])
            P.tt(tB, pa, cv[:, 5 + d, :], ALU.add)
            P.act(av[d], tB, AF.Sigmoid)
            P.stt(tB, av[d], -1.0, cv[:, 1, :], ALU.add, ALU.mult)
            P.stt(krep[d], tB, 1.0, k_, ALU.add, ALU.mult)
        pg = P.bank(5)
        P.mm(pg, lT[:, 128:256], G2)
        P.copy(xout[:, 1, :], pg, eng="act")
        P.tt(tA, krep[0], krep[1], ALU.add)
        P.tt(tA, tA, r_, ALU.mult)
        P.tt(tA, tA, cv[:, 2, :], ALU.mult)
        P.reduce(s8, h3(tA), ALU.add)
        P.tt(h3(xout[:, 0, :]), h3(v_), b8(s8), ALU.mult)
        P.dma("sp", T.RWX[R0:R0 + 128, :, :], xout)
        py0 = P.bank(7)
        for d in range(2):
            P.tt(bb, kk, av[d], ALU.mult)
            pc = P.bank(6)
            P.mm(pc, mk[:, d, :], lw[d])
            P.copy(cums, pc, eng="act")
            P.mm(pc, mk[:, 2, :], lw[d])
            P.act(pe_[0], cums, AF.Exp)
            P.act(pe_[1], cums, AF.Exp, scale=-1.0)
            P.tt(tA, cums, lw[d], ALU.subtract)
            P.act(pe_[2], tA, AF.Exp)
            P.tt(tB, pc, cums, ALU.subtract)
            P.act(pe_[3], tB, AF.Exp)
            P.stt(Z[:, 0, :], kk, -1.0, pe_[2], ALU.mult, ALU.mult)
            P.tt(Z[:, 1, :], r_, pe_[0], ALU.mult)
            P.tt(Z[:, 2, :], bb, pe_[1], ALU.mult)
            P.tt(Z[:, 3, :], krep[d], pe_[1], ALU.mult, eng="pool")
            P.tt(Z[:, 4, :], bb, pe_[3], ALU.mult)
            P.tt(Z[:, 5, :], krep[d], pe_[3], ALU.mult, eng="pool")
            P.tt(ptot, pe_[3][0:64, :], pe_[0][0:64, :], ALU.mult)
            for q in range(4):
                pz = P.bank(q, BF16)
                for h in range(8):
                    P.tr(pz[0:64, h * 128:(h + 1) * 128], Z[:, q, h * 64:(h + 1) * 64], T.identb)
                P.copy(ZT[:, :, q, :], pz[0:64, 0:1024].rearrange("p (h t) -> p h t", h=8),
                       eng=("act" if q % 2 else "dve"))
            sm, im = mk[:, 3 + 2 * d, :], mk[:, 4 + 2 * d, :]
            smT = mk[:, 5 - 2 * d, :]
            mb = lambda m_: m_.unsqueeze(1).to_broadcast([128, 8, 128])
            p1 = P.bank(0, n=4)
            p1v = p1.rearrange("p (h a t) -> p h a t", h=8, a=2)
            for h in range(8):
                P.mm(p1[:, h * 256:(h + 1) * 256], ZT[:, h, 2, :], ZT[:, h, 0:2, :].rearrange("p a t -> p (a t)"))
            p3 = P.bank(4, n=2)
            for h in range(8):
                P.mm(p3[:, h * 128:(h + 1) * 128], ZT[:, h, 0, :], ZT[:, h, 2, :])
            P.tt(NN, p1v[:, :, 0, :], mb(sm), ALU.mult)
            P.tt(LRB, p1v[:, :, 1, :], mb(im), ALU.mult)
            P.tt(AA, p3.rearrange("p (h t) -> p h t", h=8), mb(smT), ALU.mult)
            for h in range(8):
                P.mm(p1[:, h * 256:(h + 1) * 256], ZT[:, h, 3, :], ZT[:, h, 0:2, :].rearrange("p a t -> p (a t)"))
            P.tt(AAK, p1v[:, :, 0, :], mb(sm), ALU.mult)
            P.tt(LRK, p1v[:, :, 1, :], mb(im), ALU.mult)
            P.tt(MT, NN, T.identf.unsqueeze(1).to_broadcast([128, 8, 128]), ALU.add)
            nk, ak = NN, AA
            for lev in range(6):
                pn = P.bank(0, n=2)
                pa2 = P.bank(2, n=2)
                for h in range(8):
                    P.mm(pn[:, h * 128:(h + 1) * 128], ak[:, h, :], nk[:, h, :])
                for h in range(8):
                    P.mm(pa2[:, h * 128:(h + 1) * 128], nk[:, h, :], ak[:, h, :])
                nk2, ak2 = Nk[lev % 2], Ak[lev % 2]
                P.copy(nk2.rearrange("p h t -> p (h t)"), pn, eng="act")
                P.copy(ak2.rearrange("p h t -> p (h t)"), pa2, eng="dve")
                nk, ak = nk2, ak2
                pm = P.bank(4, n=2)
                for h in range(8):
                    P.mm(pm[:, h * 128:(h + 1) * 128], ak[:, h, :], MT[:, h, :])
                P.tt(MT.rearrange("p h t -> p (h t)"), MT.rearrange("p h t -> p (h t)"), pm, ALU.add)
            P.copy(MTb, MT, eng="act")
            px = P.bank(0)
            for h in range(8):
                P.mm(px[:, h * 64:(h + 1) * 64], AAK[:, h, :], vb[:, h * 64:(h + 1) * 64])
            P.copy(ZX[:, :, 0:64], h3(Z[:, 0, :]), eng="pool")
            P.copy(ZX[:, :, 64:128], px.rearrange("p (h d) -> p h d", h=8), eng="act")
            pwu = P.bank(1, n=2)
            for h in range(8):
                P.mm(pwu[:, h * 128:(h + 1) * 128], MTb[:, h, :], ZX[:, h, :])
            P.copy(WU.rearrange("p h t -> p (h t)"), pwu, eng="act")
            pq = P.bank(3, n=2)
            for h in range(8):
                P.mm(pq[0:64, h * 128:(h + 1) * 128], WU[:, h, 0:64], LRB[:, h, :])
            P.tt(QE, pq[0:64, :].rearrange("p (h t) -> p h t", h=8), ZT[:, :, 1, :], ALU.add)
            P.dma("sp", T.RQE[d, i], QE.rearrange("p h t -> p (h t)"))
            for h in range(8):
                hs = slice(h * 64, (h + 1) * 64)
                P.mm(py0[:, hs], LRB[:, h, :], WU[:, h, 64:128], start=True, stop=False)
                P.mm(py0[:, hs], LRK[:, h, :], vb[:, hs], start=False, stop=True)
            if d == 0:
                P.copy(Y0s, py0, eng="act")
            else:
                P.tt(Y0s, Y0s, py0, ALU.add)
            pgt = P.bank(5)
            ph0 = P.bank(6)
            for h in range(8):
                hs = slice(h * 64, (h + 1) * 64)
                P.mm(pgt[0:64, hs], WU[:, h, 0:64], Z[:, 4, hs])
            for h in range(8):
                hs = slice(h * 64, (h + 1) * 64)
                P.mm(ph0[0:64, hs], Z[:, 4, hs], WU[:, h, 64:128], start=True, stop=False)
                P.mm(ph0[0:64, hs], Z[:, 5, hs], vb[:, hs], start=False, stop=True)
            P.tt(dg, id64.unsqueeze(1).to_broadcast([64, 8, 64]), ptot.rearrange("p (h d) -> p h d", h=8), ALU.mult)
            P.tt(GTs, pgt[0:64, :], dg.rearrange("p h d -> p (h d)"), ALU.add)
            P.copy(H0s, ph0[0:64, :], eng="act")
            P.dma("sp", T.RGT[d, i], GTs)
            P.dma("sp", T.RH0[d, i], H0s)
            if getattr(cfg, "dbg", None) == (d, i):
                for n_, tl in enumerate([LRB, LRB, AAK, LRK, LRB, MTb, ZX, WU]):
                    P.dma("sp", T.DBG[n_], tl.rearrange("p h t -> p (h t)"))
                P.dma("sp", T.DBGZ, Z.rearrange("p a c -> p (a c)"))
                P.dma("sp", T.DBGZT, ZT.rearrange("p h a t -> p (h a t)"))
        P.dma("sp", T.RY0[i], Y0s)
    P.barrier()
    P.release(m0)
    T0 = P.sb("T0", [64, 2, NT + 1, 512], BF16)
    Tm = P.sb("Tm", [64, 512], F32)
    gts = [P.sb("gts%d" % a, [64, 512], BF16) for a in range(3)]
    h0s = [P.sb("h0s%d" % a, [64, 512], BF16) for a in range(3)]
    orders = [list(range(NT)), list(range(NTC - 1, -1, -1)) + list(range(NT - 1, NTC - 1, -1))]
    slot_of = {}
    for d in range(2):
        P.memset(Tm, 0.0)
        cur = NT
        P.memset(T0[:, d, NT, :], 0.0)
        for n_, c in enumerate(orders[d]):
            slot_of[(d, c)] = cur
            g_, h_ = gts[n_ % 3], h0s[n_ % 3]
            P.dma("sp", g_, T.RGT[d, c])
            P.dma("act", h_, T.RH0[d, c])
            ps = P.bank((n_ + d) % 2)
            for h in range(8):
                hs = slice(h * 64, (h + 1) * 64)
                P.mm(ps[0:64, hs], g_[:, hs], T0[:, d, cur, hs])
            P.tt(Tm, ps[0:64, :], h_, ALU.add)
            P.copy(T0[:, d, c, :], Tm, eng="act")
            cur = c
    lng = P.sb("lng", [128, 2, 512], F32)
    P.dma("sp", lng[:, 0, :], bc(T.rwkv_ln_g[li:li + 1, :], 512))
    P.dma("sp", lng[:, 1, :], bc(T.rwkv_ln_b[li:li + 1, :], 512))
    qes = [P.sb("qes%d" % a, [64, 2, 1024], BF16) for a in range(2)]
    y0 = [P.sb("y0_%d" % a, [128, 512], F32) for a in range(2)]
    xo = [P.sb("xo%d" % a, [128, 2, 512], F32) for a in range(2)]
    y = P.sb("yw", [128, 512], F32)
    ysq = P.sb("ywsq", [128, 512], F32)
    st = P.sb("st", [128, 4, 8], F32)
    yo = P.sb("ywo", [128, 512], BF16)
    yT = [P.sb("ywT%d" % a, [128, 4, 128], BF16) for a in range(2)]
    for c in range(NT):
        if not (want_ctx or c >= NTC):
            continue
        q_, y0_, xo_ = qes[c % 2], y0[c % 2], xo[c % 2]
        P.dma("sp", q_[:, 0, :], T.RQE[0, c])
        P.dma("sp", q_[:, 1, :], T.RQE[1, c])
        P.dma("act", y0_, T.RY0[c])
        P.dma("act", xo_, T.RWX[c * 128:(c + 1) * 128, :, :])
        py = P.bank(2 + c % 2)
        for h in range(8):
            hs = slice(h * 64, (h + 1) * 64)
            for d in range(2):
                P.mm(py[:, hs], q_[:, d, h * 128:(h + 1) * 128], T0[:, d, slot_of[(d, c)], hs],
                     start=(d == 0), stop=(d == 1))
        P.tt(y, py, y0_, ALU.add)
        P.reduce(st[:, 0, :], h3(y), ALU.add)
        P.tt(ysq, y, y, ALU.mult)
        P.reduce(st[:, 1, :], h3(ysq), ALU.add)
        P.ts(st[:, 0, :], st[:, 0, :], 1.0 / 64, None, ALU.mult)
        P.tt(st[:, 2, :], st[:, 0, :], st[:, 0, :], ALU.mult)
        P.stt(st[:, 1, :], st[:, 1, :], 1.0 / 64, st[:, 2, :], ALU.mult, ALU.subtract)
        P.act(st[:, 1, :], st[:, 1, :], AF.Sqrt, bias=64e-5)
        P.recip(st[:, 1, :], st[:, 1, :])
        P.tt(h3(y), h3(y), b8(st[:, 0, :]), ALU.subtract)
        P.tt(h3(y), h3(y), b8(st[:, 1, :]), ALU.mult)
        P.tt(y, y, lng[:, 0, :], ALU.mult)
        P.tt(y, y, lng[:, 1, :], ALU.add)
        P.tt(y, y, xo_[:, 0, :], ALU.add)
        P.tt(yo, y, xo_[:, 1, :], ALU.mult)
        pt = P.bank(4 + c % 2, BF16)
        for a in range(4):
            P.tr(pt[:, a * 128:(a + 1) * 128], yo[:, a * 128:(a + 1) * 128], T.identb)
        yt = yT[c % 2]
        P.copy(yt.rearrange("p a t -> p (a t)"), pt[:, 0:512], eng="act")
        P.dma("sp", T.YWT[:, :, c * 128:(c + 1) * 128].rearrange("a p t -> p a t"), yt)
    P.barrier()
    P.release(m0)


def phase_merge(P, cfg, T, l, li):
    m0 = P.mark()
    NT, NTC = cfg.NT, cfg.NTC
    WA = P.sb("WbrA", [64, 8, D], BF16)
    WR_ = P.sb("WbrR", [128, 4, D], BF16)
    WW = P.sb("WbrW", [128, 4, D], BF16)
    WO = P.sb("WoutB", [128, 8, D], BF16)
    for h in range(8):
        P.dma("pool", WA[:, h, :], T.w_branch[li, 0, h * 64:(h + 1) * 64, :])
    for a in range(4):
        P.dma("pool", WR_[:, a, :], T.w_branch[li, 1, a * 128:(a + 1) * 128, :])
        P.dma("pool", WW[:, a, :], T.w_branch[li, 2, a * 128:(a + 1) * 128, :])
    for kt in range(8):
        P.dma("pool", WO[:, kt, :], T.w_out[li, kt * 128:(kt + 1) * 128, :])
    G1 = load_mod(P, T, l, 2, "g1")
    ya = [P.sb("mya%d" % s_, [64, 8, 128], BF16) for s_ in range(2)]
    yr = [P.sb("myr%d" % s_, [128, 4, 128], BF16) for s_ in range(2)]
    yw = [P.sb("myw%d" % s_, [128, 4, 128], BF16) for s_ in range(2)]
    gl = [P.sb("mgl%d" % s_, [128, 3 * D], BF16) for s_ in range(2)]
    gs = P.sb("mgs", [128, 3 * D], F32)
    mg = P.sb("mmg", [128, D], F32)
    tm = P.sb("mtm", [128, D], F32)
    mb = P.sb("mmb", [128, D], BF16)
    mT = P.sb("mmT", [128, 8, 128], BF16)
    xs = [P.sb("mx%d" % s_, [128, D], F32) for s_ in range(2)]
    for i in range(NT):
        cs_ = slice(i * 128, (i + 1) * 128)
        a_, r_, w_, g_, x_ = ya[i % 2], yr[i % 2], yw[i % 2], gl[i % 2], xs[i % 2]
        P.dma("sp", a_, T.YAT[:, :, cs_].rearrange("h d t -> d h t"))
        P.dma("sp", r_, T.YRT[:, :, cs_].rearrange("a p t -> p a t"))
        P.dma("sp", w_, T.YWT[:, :, cs_].rearrange("a p t -> p a t"))
        P.dma("act", g_, T.U[cs_, 4608:7680])
        P.dma("act", x_, T.XS[cs_, :])
        P.act(gs, g_, AF.Sigmoid)
        for n in range(3):
            pp = P.bank(2 * (n % 2), n=2)
            for cc in range(2):
                cols = slice(cc * 512, (cc + 1) * 512)
                if n == 0:
                    for h in range(8):
                        P.mm(pp[:, cols], a_[:, h, :], WA[:, h, cols], start=(h == 0), stop=(h == 7))
                else:
                    src, wt = (r_, WR_) if n == 1 else (w_, WW)
                    for a in range(4):
                        P.mm(pp[:, cols], src[:, a, :], wt[:, a, cols], start=(a == 0), stop=(a == 3))
            if n == 0:
                P.tt(mg, pp, gs[:, 0:D], ALU.mult)
            else:
                P.tt(tm, pp, gs[:, n * D:(n + 1) * D], ALU.mult)
                P.tt(mg, mg, tm, ALU.add)
        P.copy(mb, mg, eng="act")
        pt = P.bank(6, BF16)
        for kt in range(8):
            P.tr(pt[:, kt * 128:(kt + 1) * 128], mb[:, kt * 128:(kt + 1) * 128], T.identb)
        P.copy(mT.rearrange("p k t -> p (k t)"), pt[:, 0:1024], eng="act")
        po = P.bank(4, n=2)
        for cc in range(2):
            cols = slice(cc * 512, (cc + 1) * 512)
            for kt in range(8):
                P.mm(po[:, cols], mT[:, kt, :], WO[:, kt, cols], start=(kt == 0), stop=(kt == 7))
        P.tt(tm, po, G1[0 if i < NTC else 1], ALU.mult)
        P.tt(x_, x_, tm, ALU.add)
        P.dma("sp", T.XM[cs_, :], x_)
    P.barrier()
    P.release(m0)


def phase_moe(P, cfg, T, l, li):
    m0 = P.mark()
    NT, NTC, N = cfg.NT, cfg.NTC, cfg.N
    Wr = P.sb("Wr", [128, 8, 36], F32)
    P.dma("sp", Wr[:, :, 0:4], T.router_grp_w[li].rearrange("(kt p) c -> p kt c", p=128))
    P.dma("sp", Wr[:, :, 4:36], T.router_exp_w[li].rearrange("(kt p) c -> p kt c", p=128))
    rb = P.sb("rb", [128, 36], F32)
    P.dma("sp", rb[:, 0:4], T.router_grp_b[li:li + 1, :].to_broadcast([128, 4]))
    P.dma("sp", rb[:, 4:36], T.router_exp_b[li:li + 1, :].to_broadcast([128, 32]))
    WR = P.sb("WRt", [128, NT, 32], F32)
    mA = P.mark()
    G2 = load_mod(P, T, l, 4, "G2")
    SH2 = load_mod(P, T, l, 3, "SH2")
    xs = [P.sb("ex%d" % s_, [128, D], F32) for s_ in range(2)]
    hf = P.sb("ehf", [128, D], F32)
    hbj = P.sb("ehb", [128, D], BF16)
    h2Tf = P.sb("eh2Tf", [128, 8, 128], F32)
    h2Tb = [P.sb("eh2Tb%d" % s_, [128, 8, 128], BF16) for s_ in range(2)]
    ss = P.sb("ess", [128, 1], F32)
    rstd = P.sb("erstd", [128, 1], F32)
    lg = P.sb("elg", [128, 36], F32)
    sm = P.sb("esm", [128, 16], F32)
    gmask = P.sb("egm", [128, 4], F32)
    e4 = P.sb("ee4", [128, 4], F32)
    sel = P.sb("esel", [128, 4, 8], F32)
    elg = P.sb("eelg", [128, 8], F32)
    srt = P.sb("esrt", [128, 8], F32)
    m8 = P.sb("em8", [128, 8], F32)
    e8 = P.sb("ee8", [128, 8], F32)
    for i in range(NT):
        cs_ = slice(i * 128, (i + 1) * 128)
        x_ = xs[i % 2]
        isl = 0 if i < NTC else 1
        import os
        stp = os.environ.get("MOE_A", "9")
        if stp == "0":
            continue
        P.dma("sp", x_, T.XM[cs_, :])
        rms_mod_tile(P, x_, G2[isl], SH2[isl], hf, hbj, ss, rstd)
        P.tt(hf, hf, SH2[isl], ALU.add)
        if stp == "0.5":
            continue
        ptf = P.bank(0, n=2)
        for kt in range(8):
            P.tr(ptf[:, kt * 128:(kt + 1) * 128], hf[:, kt * 128:(kt + 1) * 128], T.identf)
        P.copy(h2Tf.rearrange("p k t -> p (k t)"), ptf, eng="act")
        hb = h2Tb[i % 2]
        P.copy(hb.rearrange("p k t -> p (k t)"), h2Tf.rearrange("p k t -> p (k t)"), eng="dve")
        P.dma("sp", T.H2T[:, :, cs_], hb)
        import os
        stp = os.environ.get("MOE_A", "9")
        if stp == "1":
            continue
        pl = P.bank(2)
        for kt in range(8):
            P.mm(pl[:, 0:36], h2Tf[:, kt, :], Wr[:, kt, :], start=(kt == 0), stop=(kt == 7))
        P.tt(lg, pl[:, 0:36], rb, ALU.add)
        P.reduce(sm[:, 0:1], lg[:, 0:4], ALU.max)
        P.ts(gmask, lg[:, 0:4], sm[:, 0:1], None, ALU.is_equal)
        P.ts(sm[:, 1:2], sm[:, 0:1], -1.0, None, ALU.mult)
        P.act(e4, lg[:, 0:4], AF.Exp, bias=sm[:, 1:2])
        P.reduce(sm[:, 2:3], e4, ALU.add)
        P.recip(sm[:, 2:3], sm[:, 2:3])
        if stp == "2":
            continue
        el = lg[:, 4:36].rearrange("p (g e) -> p g e", g=4)
        P.tt(sel, el, gmask.unsqueeze(2).to_broadcast([128, 4, 8]), ALU.mult)
        P.reduce(elg, sel.rearrange("p g e -> p e g"), ALU.add)
        P.generic("dve", (lambda e, o_=_ap(srt), i_=_ap(elg): e.max(o_, i_)), [elg], [srt])
        P.ts(m8, elg, srt[:, 1:2], None, ALU.is_ge)
        P.ts(sm[:, 3:4], srt[:, 0:1], -1.0, None, ALU.mult)
        P.act(e8, elg, AF.Exp, bias=sm[:, 3:4])
        P.tt(e8, e8, m8, ALU.mult)
        P.reduce(sm[:, 4:5], e8, ALU.add)
        P.recip(sm[:, 4:5], sm[:, 4:5])
        P.tt(sm[:, 5:6], sm[:, 4:5], sm[:, 2:3], ALU.mult)
        P.ts(e8, e8, sm[:, 5:6], None, ALU.mult)
        P.tt(WR[:, i, :].rearrange("p (g e) -> p g e", g=4), gmask.unsqueeze(2).to_broadcast([128, 4, 8]),
             e8.unsqueeze(1).to_broadcast([128, 4, 8]), ALU.mult)
    P.barrier()
    P.release(mA)
    import os
    if os.environ.get("MOE_STOP") == "1":
        P.release(m0)
        return
    TH = 17
    g2m = load_mod(P, T, l, 5, "g2m")
    wg = [P.sb("wg%d" % s_, [128, 8, 512], BF16) for s_ in range(2)]
    wu = [P.sb("wu%d" % s_, [128, 8, 512], BF16) for s_ in range(2)]
    wd = [P.sb("wd%d" % s_, [128, 4, D], BF16) for s_ in range(2)]
    nth = min(TH, NT)
    H2 = P.sb("H2", [128, 8, nth * 128], BF16)
    yacc = P.sb("yacc", [128, nth, D], F32)
    HT = P.sb("HT", [128, 4, nth * 128], BF16)
    sg = [P.sb("esg%d" % s_, [128, 512], F32) for s_ in range(2)]
    xo = [P.sb("exo%d" % s_, [128, D], F32) for s_ in range(2)]
    ne = 0
    for t0 in range(0, NT, TH):
        nt = min(TH, NT - t0)
        ntok = nt * 128
        P.dma("sp", H2[:, :, 0:ntok], T.H2T[:, :, t0 * 128:t0 * 128 + ntok])
        P.memset(yacc[:, 0:nt, :], 0.0, eng="pool")
        for e in range(32):
            g_, u_, d_ = wg[ne % 2], wu[ne % 2], wd[ne % 2]
            ne += 1
            gv = T.moe_w_gate[li, e].rearrange("(kt p) c -> p kt c", p=128)
            uv = T.moe_w_up[li, e].rearrange("(kt p) c -> p kt c", p=128)
            dv = T.moe_w_down[li, e].rearrange("(a p) c -> p a c", p=128)
            for kt in range(8):
                P.dma("pool", g_[:, kt, :], gv[:, kt, :])
                P.dma("pool", u_[:, kt, :], uv[:, kt, :])
            for a in range(4):
                P.dma("pool", d_[:, a, :], dv[:, a, :])
            nb = 0
            for ht in range(4):
                hsl = slice(ht * 128, (ht + 1) * 128)
                for c0 in range(0, ntok, 512):
                    cw = min(512, ntok - c0)
                    pg = P.bank(2 * (nb % 2))
                    pu = P.bank(2 * (nb % 2) + 1)
                    s_ = sg[nb % 2]
                    nb += 1
                    for kt in range(8):
                        P.mm(pg[:, 0:cw], g_[:, kt, hsl], H2[:, kt, c0:c0 + cw], start=(kt == 0), stop=(kt == 7))
                    for kt in range(8):
                        P.mm(pu[:, 0:cw], u_[:, kt, hsl], H2[:, kt, c0:c0 + cw], start=(kt == 0), stop=(kt == 7))
                    P.act(s_[:, 0:cw], pg[:, 0:cw], AF.Silu)
                    P.tt(HT[:, ht, c0:c0 + cw], s_[:, 0:cw], pu[:, 0:cw], ALU.mult)
            for j in range(nt):
                po = P.bank(4 + 2 * (j % 2), n=2)
                for cc in range(2):
                    cols = slice(cc * 512, (cc + 1) * 512)
                    for a in range(4):
                        P.mm(po[:, cols], HT[:, a, j * 128:(j + 1) * 128], d_[:, a, cols], start=(a == 0), stop=(a == 3))
                P.stt(yacc[:, j, :], po, WR[:, t0 + j, e:e + 1], yacc[:, j, :], ALU.mult, ALU.add)
        for j in range(nt):
            i = t0 + j
            cs_ = slice(i * 128, (i + 1) * 128)
            x_ = xo[j % 2]
            P.dma("act", x_, T.XM[cs_, :])
            P.tt(yacc[:, j, :], yacc[:, j, :], g2m[0 if i < NTC else 1], ALU.mult)
            P.tt(x_, x_, yacc[:, j, :], ALU.add)
            P.dma("sp", T.XS[cs_, :], x_)
    P.barrier()
    P.release(m0)


def build(cfg, phases=("ada", "inproj"), debug_outs=()):
    nc = bass.Bass("TRN2", target_bir_lowering=False)
    P = Prog(nc)
    T = Ctx()
    L = len(cfg.layers)
    T.x = nc.dram_tensor("x", [cfg.NL, D], F32, kind="ExternalInput").ap()
    T.ctx = nc.dram_tensor("ctx", [cfg.NC, D], F32, kind="ExternalInput").ap()
    T.c = nc.dram_tensor("c", [1, D], F32, kind="ExternalInput").ap()
    T.c_ctx = nc.dram_tensor("c_ctx", [1, D], F32, kind="ExternalInput").ap()
    for name, shp in WEIGHT_SPECS:
        setattr(T, name, nc.dram_tensor(name, [L] + list(shp), F32, kind="ExternalInput").ap())
    hc = host_consts(cfg)
    for name, arr in hc.items():
        setattr(T, "c_" + name, nc.dram_tensor("c_" + name, list(arr.shape), F32, kind="ExternalInput").ap())

    def scratch(name, shape, dt):
        kind = "ExternalOutput" if name in debug_outs else "Internal"
        return nc.dram_tensor(name, list(shape), dt, kind=kind).ap()

    T.XS = scratch("XS", [cfg.N, D], F32)
    T.MOD = scratch("MOD", [cfg.LT, 2, 6 * D], F32)
    T.U = scratch("U", [cfg.N, IN_COLS], BF16)
    T.YAT = scratch("YAT", [8, 64, cfg.N], BF16)
    T.YRT = scratch("YRT", [4, 128, cfg.N], BF16)
    T.YWT = scratch("YWT", [4, 128, cfg.N], BF16)
    T.RWX = scratch("RWX", [cfg.N, 2, 512], F32)
    T.RQE = scratch("RQE", [2, cfg.NT, 64, 1024], BF16)
    T.RY0 = scratch("RY0", [cfg.NT, 128, 512], F32)
    T.RGT = scratch("RGT", [2, cfg.NT, 64, 512], BF16)
    T.RH0 = scratch("RH0", [2, cfg.NT, 64, 512], BF16)
    T.DBG = scratch("DBG", [8, 128, 1024], BF16)
    T.DBGZ = scratch("DBGZ", [128, 3072], BF16)
    T.DBGZT = scratch("DBGZT", [64, 4096], BF16)
    T.XM = scratch("XM", [cfg.N, D], F32)
    T.H2T = scratch("H2T", [128, 8, cfg.N], BF16)
    T.out = nc.dram_tensor("out", [cfg.NL, D], F32, kind="ExternalOutput").ap()
    T.ctx_out = nc.dram_tensor("ctx_out", [cfg.NC, D], F32, kind="ExternalOutput").ap()

    T.identf = P.sb("identf", [128, 128], F32)
    T.identb = P.sb("identb", [128, 128], BF16)
    P.dma("sp", T.identf, T.c_ident)
    P.dma("pool", T.identb, T.c_ident)
    phase0(P, cfg, T)
    for li, l in enumerate(cfg.layers):
        if "ada" in phases:
            phase_ada(P, cfg, T, l, li)
        if "inproj" in phases:
            phase_inproj(P, cfg, T, l, li)
        want_ctx = (l < cfg.LT - 1) or cfg.want_ctx_last
        if "attn" in phases:
            phase_attn(P, cfg, T, l, li, want_ctx)
        if "ret" in phases:
            phase_ret(P, cfg, T, l, li, want_ctx)
        if "rwkv" in phases:
            phase_rwkv(P, cfg, T, l, li, want_ctx)
        if "merge" in phases:
            phase_merge(P, cfg, T, l, li)
        if "moe" in phases:
            phase_moe(P, cfg, T, l, li)
    P.dma("sp", T.out, T.XS[cfg.NC:cfg.N, :])
    P.dma("sp", T.ctx_out, T.XS[0:cfg.NC, :])
    fk = ["out", "ctx_out"] + list(debug_outs)
    P.emit(final_keys=fk)
    return nc, P


FUSED = True
ALL_PHASES = ("ada", "inproj", "attn", "ret", "rwkv", "merge", "moe")
_PROG_CACHE = {}


def _get_prog(key, cfg):
    if key not in _PROG_CACHE:
        _PROG_CACHE[key] = build(cfg, phases=ALL_PHASES)[0]
    return _PROG_CACHE[key]


def kernel(**inputs):
    from concourse.bass_utils import run_bass_kernel_spmd
    inp = {k: np.ascontiguousarray(np.asarray(v, dtype=np.float32)) for k, v in inputs.items()}
    B, NL, _ = inp["x"].shape
    NC = inp["ctx"].shape[1]
    depth = inp["w_in"].shape[0]
    ncores = B
    if FUSED:
        cfg = Cfg(NL=NL, NC=NC, layers=list(range(depth)), nlayers_total=depth, want_ctx_last=False)
        launches = [list(range(depth))]
    else:
        cfg = Cfg(NL=NL, NC=NC, layers=[0], nlayers_total=1, want_ctx_last=True)
        launches = [[l] for l in range(depth)]
    nc = _get_prog(("fused" if FUSED else "layer", NL, NC, depth), cfg)
    consts = {"c_" + k: v for k, v in host_consts(cfg).items()}
    xs = [inp["x"][b] for b in range(B)]
    cx = [inp["ctx"][b] for b in range(B)]
    for ls in launches:
        wl = {name: np.ascontiguousarray(inp[name][ls]) for name, _ in WEIGHT_SPECS}
        in_maps = []
        for b in range(ncores):
            m = dict(x=xs[b], ctx=cx[b], c=inp["c"][b:b + 1], c_ctx=inp["c_ctx"][None, :])
            m.update(wl)
            m.update(consts)
            in_maps.append(m)
        res = run_bass_kernel_spmd(nc, in_maps, core_ids=list(range(ncores)))
        xs = [np.asarray(r["out"]) for r in res.results]
        cx = [np.asarray(r["ctx_out"]) for r in res.results]
    return np.stack(xs).astype(np.float32)
```

```python
import contextlib
import numpy as np
import concourse.bass as bass
import concourse.mybir as mybir

F32 = mybir.dt.float32
BF16 = mybir.dt.bfloat16
I32 = mybir.dt.int32
AF = mybir.ActivationFunctionType
ALU = mybir.AluOpType
AX = mybir.AxisListType

COMPUTE = ("pe", "act", "dve", "pool")
ENGATTR = {"pe": "tensor", "act": "scalar", "dve": "vector", "pool": "gpsimd", "sp": "sync"}


class V:
    __slots__ = ("ap", "key")

    def __init__(self, ap, key):
        self.ap = ap
        self.key = key

    def __getitem__(self, idx):
        return V(self.ap[idx], self.key)

    def __getattr__(self, name):
        a = getattr(self.ap, name)
        if callable(a):
            def f(*args, **kw):
                r = a(*args, **kw)
                return V(r, self.key) if isinstance(r, type(self.ap)) else r
            return f
        return a


def _key(x):
    return x.key if isinstance(x, V) else x.tensor.name


def _flat(keys):
    out = []
    for k in keys:
        if isinstance(k, tuple):
            out.extend(k)
        else:
            out.append(k)
    return out


def _ap(x):
    return x.ap if isinstance(x, V) else x


def _isnum(x):
    return isinstance(x, (int, float))


class Prog:
    def __init__(self, nc, arena_bytes=212480):
        self.nc = nc
        self.ops = []
        self.eng_ops = {e: [] for e in ENGATTR}
        self.state = {}
        self.es = contextlib.ExitStack()
        self.last = {}
        self.pending_dma = []
        self.arena = self.es.enter_context(nc.sbuf_tensor("arena", [128, arena_bytes // 4], F32))
        self.arena_bytes = arena_bytes
        self.aptr = 0
        self.nalloc = 0
        self.psum = self.es.enter_context(nc.psum_tensor("psum_all", [128, 4096], F32))

    def sb(self, name, shape, dtype, parts=128):
        esz = mybir.dt.size(dtype)
        n = 1
        for s in shape[1:]:
            n *= s
        nbytes = (n * esz + 63) // 64 * 64
        assert self.aptr + nbytes <= self.arena_bytes, (name, self.aptr, nbytes)
        a = self.arena[0:shape[0], self.aptr // 4:(self.aptr + nbytes) // 4]
        if dtype != F32:
            a = a.bitcast(dtype)
        if a.shape[-1] != n:
            a = a[:, 0:n]
        self.aptr += nbytes
        self.nalloc += 1
        v = V(a, "%s#%d" % (name, self.nalloc))
        if len(shape) > 2:
            names = " ".join("d%d" % i for i in range(1, len(shape)))
            v = v.rearrange("p (%s) -> p %s" % (names, names),
                            **{"d%d" % i: shape[i] for i in range(1, len(shape))})
        return v

    def mark(self):
        return self.aptr

    def release(self, mark):
        self.aptr = mark

    def bank(self, i, dtype=F32, n=1):
        a = self.psum[:, 512 * i:512 * (i + n)]
        if dtype != F32:
            a = a.bitcast(dtype)
        if n == 1:
            return V(a, "psb%d" % i)
        return V(a, tuple("psb%d" % j for j in range(i, i + n)))

    def dram(self, name, shape, dtype, kind="Internal"):
        return self.nc.dram_tensor(name, list(shape), dtype, kind=kind).ap()

    def _collapse(self, deps):
        best = {}
        out = set()
        for d in deps:
            o = self.ops[d]
            if o["dma"]:
                out.add(d)
            else:
                b = best.get(o["eng"])
                if b is None or d > b:
                    best[o["eng"]] = d
        out.update(best.values())
        return out

    def op(self, eng, fn, reads=(), writes=(), dma=False):
        idx = len(self.ops)
        reads = list(dict.fromkeys(_flat(reads)))
        writes = list(dict.fromkeys(_flat(writes)))
        o = dict(eng=eng, fn=fn, dma=dma, dkey=(writes[0] if dma else None))
        self.ops.append(o)
        raw = set()
        other = set()
        for k in reads:
            st = self.state.setdefault(k, dict(w=[], r=[]))
            raw.update(st["w"])
        for k in writes:
            st = self.state.setdefault(k, dict(w=[], r=[]))
            if k in reads or st["r"]:
                other.update(st["w"])
                other.update(st["r"])
                st["pd"] = set(st["w"]) | set(st["r"])
                st["w"] = [idx]
                st["r"] = []
            else:
                other.update(st.get("pd", ()))
                st["w"].append(idx)
                if len(st["w"]) > 48:
                    st["w"] = list(self._collapse(st["w"]))
        for k in reads:
            if k not in writes:
                st = self.state[k]
                st["r"].append(idx)
                if len(st["r"]) > 48:
                    st["r"] = list(self._collapse(st["r"]))
        if not dma:
            other = {d for d in other if self.ops[d]["dma"] or self.ops[d]["eng"] != eng}
        deps = raw | other
        deps.discard(idx)
        o["deps"] = self._collapse(deps)
        self.eng_ops[eng].append(idx)
        if dma:
            self.pending_dma.append(idx)
        else:
            self.last[eng] = idx
        return idx

    def barrier(self):
        deps = set(self.last.values()) | set(self.pending_dma)
        self.nbar = getattr(self, "nbar", 0) + 1
        for e in ENGATTR:
            idx = len(self.ops)
            o = dict(eng=e, fn=None, dma=False, dkey=None, deps=set(deps), bar=self.nbar)
            self.ops.append(o)
            self.eng_ops[e].append(idx)
        self.pending_dma = []
        self.state = {}

    def dma(self, eng, out, in_, **kw):
        o_, i_ = _ap(out), _ap(in_)
        return self.op(eng, lambda e: e.dma_start(out=o_, in_=i_, **kw),
                       [_key(in_)], [_key(out)], dma=True)

    def mm(self, out, lhsT, rhs, start=True, stop=True, **kw):
        o_, l_, r_ = _ap(out), _ap(lhsT), _ap(rhs)
        return self.op("pe", lambda e: e.matmul(o_, l_, r_, start=start, stop=stop, **kw),
                       [_key(lhsT), _key(rhs)], [_key(out)])

    def tr(self, out, in_, ident):
        o_, i_, d_ = _ap(out), _ap(in_), _ap(ident)
        return self.op("pe", lambda e: e.transpose(o_, i_, d_),
                       [_key(in_), _key(ident)], [_key(out)])

    def act(self, out, in_, func, bias=None, scale=1.0, accum_out=None, eng="act"):
        rd = [_key(in_)]
        kw = {}
        if bias is not None:
            kw["bias"] = _ap(bias)
            if not _isnum(bias):
                rd.append(_key(bias))
        if not _isnum(scale):
            rd.append(_key(scale))
        kw["scale"] = _ap(scale)
        wr = [_key(out)]
        if accum_out is not None:
            kw["accum_out"] = _ap(accum_out)
            wr.append(_key(accum_out))
        o_, i_ = _ap(out), _ap(in_)
        return self.op(eng, lambda e: e.activation(o_, i_, func, **kw), rd, wr)

    def tt(self, out, in0, in1, op, eng="dve"):
        o_, a_, b_ = _ap(out), _ap(in0), _ap(in1)
        return self.op(eng, lambda e: e.tensor_tensor(o_, a_, b_, op),
                       [_key(in0), _key(in1)], [_key(out)])

    def ts(self, out, in0, s1, s2, op0, op1=None, accum_out=None, eng="dve"):
        rd = [_key(in0)]
        for s in (s1, s2):
            if s is not None and not _isnum(s):
                rd.append(_key(s))
        wr = [_key(out)]
        kw = {}
        if accum_out is not None:
            kw["accum_out"] = _ap(accum_out)
            wr.append(_key(accum_out))
        o_, a_, s1_, s2_ = _ap(out), _ap(in0), _ap(s1), _ap(s2)
        if op1 is None:
            return self.op(eng, lambda e: e.tensor_scalar(o_, a_, s1_, None, op0, **kw), rd, wr)
        return self.op(eng, lambda e: e.tensor_scalar(o_, a_, s1_, s2_, op0, op1, **kw), rd, wr)

    def stt(self, out, in0, scalar, in1, op0, op1, eng="dve"):
        rd = [_key(in0), _key(in1)]
        if not _isnum(scalar):
            rd.append(_key(scalar))
        o_, a_, s_, b_ = _ap(out), _ap(in0), _ap(scalar), _ap(in1)
        return self.op(eng, lambda e: e.scalar_tensor_tensor(o_, a_, s_, b_, op0, op1),
                       rd, [_key(out)])

    def copy(self, out, in_, eng="dve"):
        o_, i_ = _ap(out), _ap(in_)
        if eng == "act":
            return self.op(eng, lambda e: e.copy(o_, i_), [_key(in_)], [_key(out)])
        return self.op(eng, lambda e: e.tensor_copy(o_, i_), [_key(in_)], [_key(out)])

    def memset(self, out, val, eng="dve"):
        o_ = _ap(out)
        return self.op(eng, lambda e: e.memset(o_, val), [], [_key(out)])

    def reduce(self, out, in_, op, axis=AX.X, eng="dve"):
        o_, i_ = _ap(out), _ap(in_)
        return self.op(eng, lambda e: e.tensor_reduce(o_, i_, axis, op), [_key(in_)], [_key(out)])

    def recip(self, out, in_):
        o_, i_ = _ap(out), _ap(in_)
        return self.op("dve", lambda e: e.reciprocal(o_, i_), [_key(in_)], [_key(out)])

    def generic(self, eng, fn, reads, writes):
        return self.op(eng, fn, [_key(a) for a in reads], [_key(a) for a in writes])

    def emit(self, final_keys=()):
        nc = self.nc
        ops = self.ops

        def skip(o, po):
            return (not po["dma"]) and (not o["dma"]) and po["eng"] == "pe" and o["eng"] == "pe" \
                and not o.get("bar")

        observed = set()
        for o in ops:
            for d in o["deps"]:
                po = ops[d]
                if po["dma"] or skip(o, po):
                    continue
                observed.add(d)
        finals = []
        for k in final_keys:
            st = self.state.get(k)
            if st:
                for d in st["w"]:
                    finals.append(d)
                    if not ops[d]["dma"]:
                        observed.add(d)
        sems = {e: self.es.enter_context(nc.semaphore("s_" + e)) for e in COMPUTE}
        pool = []
        pcnt = []
        dsem = {}
        cnt = {e: 0 for e in COMPUTE}
        semval = {}
        epoch = 0
        for i, o in enumerate(ops):
            if o.get("bar") and o["bar"] != epoch:
                epoch = o["bar"]
                dsem = {}
            if o["dma"]:
                g = o["dkey"]
                if g not in dsem:
                    j = len(dsem)
                    if j >= len(pool):
                        pool.append(self.es.enter_context(nc.semaphore("d%d" % j)))
                        pcnt.append(0)
                    dsem[g] = j
                j = dsem[g]
                pcnt[j] += 16
                semval[i] = (pool[j], pcnt[j])
            elif i in observed:
                cnt[o["eng"]] += 1
                semval[i] = (sems[o["eng"]], cnt[o["eng"]])
        self.n_sems = 4 + len(pool)
        self.sem_max = dict(cnt)
        with nc.Block() as block:
            for ename, attr in ENGATTR.items():
                lst = self.eng_ops[ename]
                extra = finals if ename == "sp" else []
                if not lst and not extra:
                    continue

                def section(eng, ename=ename, lst=lst, extra=extra):
                    waited = {}

                    def do_waits(deps, o, defer=False):
                        need = {}
                        for d in deps:
                            po = ops[d]
                            if o is not None and skip(o, po):
                                continue
                            if d not in semval:
                                continue
                            s, v = semval[d]
                            if waited.get(s.num, 0) >= v:
                                continue
                            if need.get(s.num, (None, 0))[1] < v:
                                need[s.num] = (s, v)
                        lst_ = list(need.values())
                        for s, v in lst_:
                            waited[s.num] = v
                        first = lst_.pop(0) if (defer and lst_) else None
                        for s, v in lst_:
                            eng.wait_ge(s, v)
                        return first

                    for i in lst:
                        o = ops[i]
                        if o.get("bar"):
                            do_waits(o["deps"], o)
                            continue
                        first = do_waits(o["deps"], o, defer=True)
                        ins = o["fn"](eng)
                        if first is not None:
                            ins._wait_ge(first[0], first[1])
                        if i in semval:
                            ins.then_inc(semval[i][0], 16 if o["dma"] else 1)
                    do_waits(extra, None)

                getattr(block, attr)(section)
        self.es.close()


D = 1024
IN_COLS = 7680
EPS = 1e-6


class Cfg:
    def __init__(self, NL=4096, NC=256, layers=(0, 1, 2, 3), nlayers_total=4, want_ctx_last=False):
        self.NL, self.NC = NL, NC
        self.N = NL + NC
        self.NT = self.N // 128
        self.NTC = NC // 128
        self.layers = list(layers)
        self.LT = nlayers_total
        self.want_ctx_last = want_ctx_last


WEIGHT_SPECS = [
    ("ada_w", (D, 6 * D)), ("ada_b", (6 * D,)), ("norm1_g", (D,)), ("norm2_g", (D,)),
    ("w_in", (D, IN_COLS)), ("att_qn_g", (64,)), ("att_kn_g", (64,)), ("ret_decay_logit", (2, 4)),
    ("ret_gn_g", (512,)), ("rwkv_mu", (2, 1792)), ("rwkv_w0", (2, 512)), ("rwkv_w2", (2, 64, 512)),
    ("rwkv_a0", (2, 512)), ("rwkv_a2", (2, 64, 512)), ("rwkv_g2", (128, 512)), ("rwkv_k_k", (512,)),
    ("rwkv_k_a", (512,)), ("rwkv_r_k", (8, 64)), ("rwkv_ln_g", (512,)), ("rwkv_ln_b", (512,)),
    ("w_branch", (3, 512, D)), ("w_out", (D, D)), ("router_grp_w", (D, 4)), ("router_grp_b", (4,)),
    ("router_exp_w", (D, 32)), ("router_exp_b", (32,)), ("moe_w_gate", (32, D, 512)),
    ("moe_w_up", (32, D, 512)), ("moe_w_down", (32, 512, D)),
]


def host_consts(cfg):
    c = {}
    c["ident"] = np.eye(128, dtype=np.float32)
    for nm, hd in (("att", 64), ("ret", 128)):
        rows = cfg.NL // 64
        row = np.broadcast_to(np.arange(rows, dtype=np.float32)[:, None], (rows, 64)).reshape(-1)
        col = np.broadcast_to(np.arange(64, dtype=np.float32)[None, :], (rows, 64)).reshape(-1)
        quarter = hd // 4
        inv = (np.float32(10000.0) ** (-np.arange(quarter, dtype=np.float32) / np.float32(quarter))).astype(np.float32)
        ang = np.stack([row[:, None] * inv, col[:, None] * inv], axis=1).astype(np.float32)
        c[nm + "_cos"] = np.cos(ang).reshape(cfg.NL, 2 * quarter).astype(np.float32)
        c[nm + "_sin"] = np.sin(ang).reshape(cfg.NL, 2 * quarter).astype(np.float32)
    sidx = np.arange(128, dtype=np.float32)[:, None]
    tidx = np.arange(128, dtype=np.float32)[None, :]
    lag = tidx - sidx
    ones = np.ones((128, 128), np.float32)
    c["retc"] = np.concatenate([np.maximum(lag, 0), np.maximum(-lag, 0), (lag >= 0).astype(np.float32),
                                (lag <= 0).astype(np.float32), ones * (tidx + 1), ones * (128 - tidx)], 1).astype(np.float32)
    le = (lag >= 0).astype(np.float32); ge = (lag <= 0).astype(np.float32)
    lt = (lag > 0).astype(np.float32); gt = (lag < 0).astype(np.float32)
    c["rwmask"] = np.concatenate([le, ge, ones, lt, le, gt, ge], 1).astype(np.float32)
    c["retcol"] = np.concatenate([sidx, 127 - sidx], 1).astype(np.float32)
    return c


class Ctx:
    pass


def cast_load(P, dst, src, eng="pool"):
    n = src.shape[-1]
    step = 2048
    for a in range(0, n, step):
        b = min(n, a + step)
        P.dma(eng, dst[..., a:b], src[..., a:b])


def phase0(P, cfg, T):
    P.dma("sp", T.XS[0:cfg.NC, :], T.ctx)
    P.dma("sp", T.XS[cfg.NC:cfg.N, :], T.x)
    P.barrier()


def phase_ada(P, cfg, T, l, li):
    m0 = P.mark()
    craw = P.sb("craw", [128, 2, 8], F32)
    cT = P.sb("cT", [128, 8, 2], F32)
    P.dma("sp", craw[:, 0, :], T.c.rearrange("o (kt p) -> p (o kt)", p=128), allow_slow_non_contiguous=True)
    P.dma("sp", craw[:, 1, :], T.c_ctx.rearrange("o (kt p) -> p (o kt)", p=128), allow_slow_non_contiguous=True)
    P.act(cT.rearrange("p k t -> p t k"), craw, AF.Silu)
    mod = P.sb("mod", [2, 6 * D], F32)
    gn = P.sb("gn", [2, 2, D], F32)
    P.dma("sp", mod, T.ada_b[li:li + 1, :].to_broadcast([2, 6 * D]))
    P.dma("sp", gn[:, 0, :], T.norm1_g[li:li + 1, :].to_broadcast([2, D]))
    P.dma("sp", gn[:, 1, :], T.norm2_g[li:li + 1, :].to_broadcast([2, D]))
    wsl = [P.sb("adaw%d" % s, [128, 8, 512], F32) for s in range(2)]
    wv = T.ada_w[li].rearrange("(kt p) c -> p kt c", p=128)
    for cc in range(12):
        w = wsl[cc % 2]
        P.dma("sp" if cc % 2 == 0 else "act", w, wv[:, :, cc * 512:(cc + 1) * 512])
        ps = P.bank(cc % 2)
        for kt in range(8):
            P.mm(ps[0:2, :], cT[:, kt, :], w[:, kt, :], start=(kt == 0), stop=(kt == 7))
        P.tt(mod[:, cc * 512:(cc + 1) * 512], mod[:, cc * 512:(cc + 1) * 512], ps[0:2, :], ALU.add)
    for slot, g in ((1, 0), (4, 1)):
        P.stt(mod[:, slot * D:(slot + 1) * D], mod[:, slot * D:(slot + 1) * D], 1.0, gn[:, g, :],
              ALU.add, ALU.mult)
    P.dma("sp", T.MOD[l], mod)
    P.barrier()
    P.release(m0)


def load_mod(P, T, l, slot, name):
    out = []
    for r in (1, 0):
        t = P.sb("%s_%d" % (name, r), [128, D], F32)
        P.dma("sp", t, T.MOD[l][r:r + 1, slot * D:(slot + 1) * D].to_broadcast([128, D]))
        out.append(t)
    return out


def rsqrt_to(P, out, in_, scale, eps):
    P.act(out, in_, AF.Sqrt, bias=eps, scale=scale)
    P.recip(out, out)


def rms_mod_tile(P, x, G, sh, hf, h, ss, rstd):
    P.act(hf, x, AF.Square, accum_out=ss)
    rsqrt_to(P, rstd, ss, 1.0 / D, EPS)
    P.stt(hf, x, rstd, G, ALU.mult, ALU.mult)
    P.tt(h, hf, sh, ALU.add)


def phase_inproj(P, cfg, T, l, li):
    m0 = P.mark()
    Wb = P.sb("Wb", [128, 8, IN_COLS], BF16)
    wv = T.w_in[li].rearrange("(kt p) c -> p kt c", p=128)
    for kt in range(8):
        cast_load(P, Wb[:, kt, :], wv[:, kt, :])
    G = load_mod(P, T, l, 1, "G1")
    SH = load_mod(P, T, l, 0, "SH1")
    xs = [P.sb("xa%d" % s, [128, D], F32) for s in range(2)]
    hf = P.sb("hf", [128, D], F32)
    h = P.sb("h", [128, D], BF16)
    hT = P.sb("hT", [128, 8, 128], BF16)
    ss = P.sb("ss", [128, 1], F32)
    rstd = P.sb("rstd", [128, 1], F32)
    ust = [P.sb("ust%d" % s, [128, IN_COLS], BF16) for s in range(2)]
    pT = P.bank(0, BF16)
    nb = 0
    for i in range(cfg.NT):
        x = xs[i % 2]
        isl = 0 if i < cfg.NTC else 1
        P.dma("sp", x, T.XS[i * 128:(i + 1) * 128, :])
        rms_mod_tile(P, x, G[isl], SH[isl], hf, h, ss, rstd)
        for kt in range(8):
            P.tr(pT[:, kt * 128:(kt + 1) * 128], h[:, kt * 128:(kt + 1) * 128], T.identb)
        P.copy(hT.rearrange("p k t -> p (k t)"), pT[:, 0:1024], eng="act")
        u = ust[i % 2]
        for cc in range(IN_COLS // 512):
            ps = P.bank(1 + nb % 7)
            nb += 1
            for kt in range(8):
                P.mm(ps, hT[:, kt, :], Wb[:, kt, cc * 512:(cc + 1) * 512], start=(kt == 0), stop=(kt == 7))
            P.copy(u[:, cc * 512:(cc + 1) * 512], ps, eng=("act" if cc % 2 else "dve"))
        P.dma("sp", T.U[i * 128:(i + 1) * 128, :], u)
    P.barrier()
    P.release(m0)


def rope_apply(P, dst, src, cos, sin, tmp, nh, q):
    v = lambda a: a.rearrange("p (h a t q) -> p h a t q", h=nh, a=2, t=2, q=q)
    s5, d5 = v(src), v(dst)
    x1, x2 = s5[:, :, :, 0, :], s5[:, :, :, 1, :]
    c4 = cos.rearrange("p (a q) -> p a q", a=2).unsqueeze(1).to_broadcast([128, nh, 2, q])
    s4 = sin.rearrange("p (a q) -> p a q", a=2).unsqueeze(1).to_broadcast([128, nh, 2, q])
    t1 = tmp[0].rearrange("p (h a q) -> p h a q", h=nh, a=2, q=q)
    t2 = tmp[1].rearrange("p (h a q) -> p h a q", h=nh, a=2, q=q)
    P.tt(t1, x1, c4, ALU.mult)
    P.tt(t2, x2, s4, ALU.mult)
    P.tt(d5[:, :, :, 0, :], t1, t2, ALU.subtract)
    P.tt(t1, x2, c4, ALU.mult)
    P.tt(t2, x1, s4, ALU.mult)
    P.tt(d5[:, :, :, 1, :], t1, t2, ALU.add)


def phase_attn(P, cfg, T, l, li, want_ctx):
    m0 = P.mark()
    NT, NTC, N = cfg.NT, cfg.NTC, cfg.N
    QT = P.sb("QT", [64, NT, 8, 128], BF16)
    KT = P.sb("KT", [64, 2, N], BF16)
    Vg = P.sb("Vg", [128, NT, 2, 65], BF16)
    gains = P.sb("gains", [128, 10, 64], F32)
    P.dma("sp", gains[:, 0, :], T.att_qn_g[li:li + 1, :].to_broadcast([128, 64]))
    P.dma("sp", gains[:, 8, :], T.att_kn_g[li:li + 1, :].to_broadcast([128, 64]))
    for hh in range(1, 8):
        P.copy(gains[:, hh, :], gains[:, 0, :], eng="pool")
    P.copy(gains[:, 9, :], gains[:, 8, :], eng="pool")
    P.memset(Vg[:, :, :, 64:65], 1.0, eng="pool")
    onesr = P.sb("onesr", [65, 64], F32)
    P.memset(onesr, 1.0, eng="pool")
    qkv = [P.sb("qkv%d" % s, [128, 768], BF16) for s in range(2)]
    cs = [P.sb("cs%d" % s, [128, 2, 32], F32) for s in range(2)]
    sq = P.sb("sq", [128, 640], F32)
    ssq = P.sb("ssq", [128, 10], F32)
    qn = P.sb("qn", [128, 640], F32)
    qr = P.sb("qr", [128, 640], BF16)
    tmp = [P.sb("rt%d" % s, [128, 320], F32) for s in range(2)]
    pTa = P.bank(6, BF16)
    pTb = P.bank(7, BF16)
    for i in range(NT):
        t = qkv[i % 2]
        P.dma("sp", t, T.U[i * 128:(i + 1) * 128, 0:768])
        P.tt(sq, t[:, 0:640], t[:, 0:640], ALU.mult)
        P.reduce(ssq, sq.rearrange("p (h d) -> p h d", h=10), ALU.add)
        rsqrt_to(P, ssq, ssq, 1.0 / 64, EPS)
        qn3 = qn.rearrange("p (h d) -> p h d", h=10)
        P.tt(qn3, t[:, 0:640].rearrange("p (h d) -> p h d", h=10),
             ssq.unsqueeze(2).to_broadcast([128, 10, 64]), ALU.mult)
        if i >= NTC:
            c = cs[i % 2]
            t0 = (i - NTC) * 128
            P.dma("act", c[:, 0, :], T.c_att_cos[t0:t0 + 128, :])
            P.dma("act", c[:, 1, :], T.c_att_sin[t0:t0 + 128, :])
            P.tt(qn3, qn3, gains, ALU.mult)
            rope_apply(P, qr, qn, c[:, 0, :], c[:, 1, :], tmp, 10, 16)
        else:
            P.tt(qr.rearrange("p (h d) -> p h d", h=10), qn3, gains, ALU.mult)
        for hh in range(8):
            P.tr(pTa[0:64, hh * 128:(hh + 1) * 128], qr[:, hh * 64:(hh + 1) * 64], T.identb)
        for kv in range(2):
            P.tr(pTb[0:64, kv * 128:(kv + 1) * 128], qr[:, 512 + kv * 64:512 + (kv + 1) * 64], T.identb)
        P.copy(QT[:, i, :, :].rearrange("p h t -> p (h t)"), pTa[0:64, 0:1024], eng="act")
        P.copy(KT[:, :, i * 128:(i + 1) * 128], pTb[0:64, 0:256].rearrange("p (k t) -> p k t", k=2), eng="act")
        P.copy(Vg[:, i, :, 0:64], t[:, 640:768].rearrange("p (k d) -> p k d", k=2), eng="pool")
    pts = [P.sb("pt%d" % s, [128, 1024], BF16) for s in range(2)]
    rsb = P.sb("rsb", [65, 512], F32)
    bcs = P.sb("bcs", [64, 512], F32)
    yts = [P.sb("yt%d" % s, [64, 512], BF16) for s in range(2)]
    it = 0
    grp = 0
    qtiles = list(range(NTC, NT)) + (list(range(NTC)) if want_ctx else [])
    for qi in qtiles:
        nk = NT if qi >= NTC else NTC
        for g in range(2):
            ob = P.bank(4 + it % 2)
            rhsq = QT[:, qi, 4 * g:4 * g + 4, :].rearrange("p h t -> p (h t)")
            for j0 in range(0, nk, 2):
                sb2 = P.bank(2 * (grp % 2), n=2)
                pt = pts[grp % 2]
                grp += 1
                nj = min(2, nk - j0)
                for jj in range(nj):
                    j = j0 + jj
                    P.mm(sb2[:, jj * 512:(jj + 1) * 512], KT[:, g, j * 128:(j + 1) * 128], rhsq)
                P.act(pt[:, 0:512 * nj], sb2[:, 0:512 * nj], AF.Exp, scale=0.125)
                for jj in range(nj):
                    j = j0 + jj
                    P.mm(ob[0:65, :], Vg[:, j, g, :], pt[:, jj * 512:(jj + 1) * 512],
                         start=(j == 0), stop=(j == nk - 1))
            P.recip(rsb[64:65, :], ob[64:65, :])
            bc = P.bank(6)
            P.mm(bc[0:64, :], onesr[64:65, :], rsb[64:65, :])
            P.copy(bcs, bc[0:64, :], eng="act")
            yt = yts[it % 2]
            P.tt(yt, ob[0:64, :], bcs, ALU.mult)
            P.dma("sp", T.YAT[4 * g:4 * g + 4, :, qi * 128:(qi + 1) * 128].rearrange("h d t -> d h t"),
                  yt.rearrange("p (h t) -> p h t", h=4))
            it += 1
    P.barrier()
    P.release(m0)


def phase_ret(P, cfg, T, l, li, want_ctx):
    m0 = P.mark()
    NT, NTC, N = cfg.NT, cfg.NTC, cfg.N
    SC = 128.0 ** -0.5
    rc = P.sb("retc", [128, 6, 128], F32)
    P.dma("sp", rc, T.c_retc.rearrange("p (a t) -> p a t", a=6))
    cols = P.sb("retcol", [128, 2], F32)
    P.dma("sp", cols, T.c_retcol)
    lg = P.sb("lg", [128, 8], F32)
    P.dma("sp", lg, T.ret_decay_logit[li:li + 1].rearrange("o a h -> o (a h)").to_broadcast([128, 8]))
    P.act(lg, lg, AF.Exp, scale=-1.0)
    P.act(lg, lg, AF.Ln, bias=1.0)
    P.ts(lg, lg, -1.0, None, ALU.mult)
    DT = P.sb("DT", [128, 4, 128], F32)
    DQ = P.sb("DQ", [128, 2, 4, 128], F32)
    e1 = P.sb("e1", [128, 128], F32)
    for h in range(4):
        P.act(e1, rc[:, 0, :], AF.Exp, scale=lg[:, h:h + 1])
        P.tt(DT[:, h, :], e1, rc[:, 2, :], ALU.mult)
        P.act(e1, rc[:, 1, :], AF.Exp, scale=lg[:, 4 + h:5 + h])
        P.tt(e1, e1, rc[:, 3, :], ALU.mult)
        P.stt(DT[:, h, :], DT[:, h, :], SC, e1, ALU.mult, ALU.mult) if False else None
        P.tt(DT[:, h, :], DT[:, h, :], e1, ALU.add)
        P.act(DQ[:, 0, h, :], rc[:, 4, :], AF.Exp, scale=lg[:, h:h + 1])
        P.act(DQ[:, 1, h, :], rc[:, 5, :], AF.Exp, scale=lg[:, 4 + h:5 + h])
    P.ts(DT, DT, SC, None, ALU.mult)
    dk = P.sb("dk", [128, 2, 4], F32)
    for h in range(4):
        P.act(dk[:, 0, h:h + 1], cols[:, 1:2], AF.Exp, scale=lg[:, h:h + 1])
        P.act(dk[:, 1, h:h + 1], cols[:, 0:1], AF.Exp, scale=lg[:, 4 + h:5 + h])
    P.ts(dk, dk, SC, None, ALU.mult)
    gC = P.sb("gC", [128, 8], F32)
    P.act(gC, lg, AF.Exp, scale=128.0)
    gng = P.sb("gng", [128, 512], F32)
    P.dma("sp", gng, T.ret_gn_g[li:li + 1, :].to_broadcast([128, 512]))

    import os
    stop = int(os.environ.get("RET_STOP", "9"))
    if stop <= 1:
        P.barrier(); P.release(m0); return
    Krot = P.sb("Krot", [128, NT, 512], BF16)
    Vs = P.sb("Vs", [128, NT, 512], BF16)
    Sf = P.sb("Sf", [128, NT + 1, 512], BF16)
    Sm = P.sb("Sm", [128, 512], F32)
    Sbb = P.sb("Sbb", [128, 512], BF16)
    kin = [P.sb("kin%d" % s, [128, 512], BF16) for s in range(2)]
    cs = [P.sb("rcs%d" % s, [128, 2, 64], F32) for s in range(2)]
    tmp = [P.sb("rrt%d" % s, [128, 256], F32) for s in range(2)]
    Kd = P.sb("Kd", [128, 512], BF16)
    P.memset(Sm, 0.0)
    P.memset(Sf[:, 0, :], 0.0)
    for c in range(NT):
        k = kin[c % 2]
        P.dma("sp", k, T.U[c * 128:(c + 1) * 128, 1280:1792])
        P.dma("act", Vs[:, c, :], T.U[c * 128:(c + 1) * 128, 1792:2304])
        if c >= NTC:
            cc = cs[c % 2]
            t0 = (c - NTC) * 128
            P.dma("sp", cc[:, 0, :], T.c_ret_cos[t0:t0 + 128, :])
            P.dma("sp", cc[:, 1, :], T.c_ret_sin[t0:t0 + 128, :])
            rope_apply(P, Krot[:, c, :], k, cc[:, 0, :], cc[:, 1, :], tmp, 4, 32)
        else:
            P.copy(Krot[:, c, :], k)
        P.tt(Kd.rearrange("p (h d) -> p h d", h=4), Krot[:, c, :].rearrange("p (h d) -> p h d", h=4),
             dk[:, 0, :].unsqueeze(2).to_broadcast([128, 4, 128]), ALU.mult)
        ps = P.bank(c % 2)
        for h in range(4):
            P.mm(ps[:, h * 128:(h + 1) * 128], Kd[:, h * 128:(h + 1) * 128], Vs[:, c, h * 128:(h + 1) * 128])
        Sm3 = Sm.rearrange("p (h d) -> p h d", h=4)
        P.tt(Sm3, Sm3, gC[:, 0:4].unsqueeze(2).to_broadcast([128, 4, 128]), ALU.mult)
        P.tt(Sm, Sm, ps, ALU.add)
        P.copy(Sf[:, c + 1, :], Sm, eng="act")
    if stop <= 2:
        P.barrier(); P.release(m0); return
    P.memset(Sm, 0.0)
    P.memset(Sbb, 0.0)
    qg = [P.sb("qg%d" % s, [128, 2, 512], BF16) for s in range(2)]
    Qr = P.sb("Qr", [128, 512], BF16)
    QTs = P.sb("QTs", [128, 3, 512], BF16)
    KTs = P.sb("KTs", [128, 512], BF16)
    AT = P.sb("AT", [128, 512], BF16)
    yf = P.sb("yf", [128, 512], F32)
    ysq = P.sb("ysq", [128, 512], F32)
    rs = P.sb("rrs", [128, 4], F32)
    sg = P.sb("sg", [128, 512], F32)
    yo = P.sb("yo", [128, 512], BF16)
    yT = [P.sb("yT%d" % s, [128, 4, 128], BF16) for s in range(2)]
    order = list(range(NTC - 1, -1, -1)) + list(range(NT - 1, NTC - 1, -1))
    for n_, c in enumerate(order):
        need_out = want_ctx or c >= NTC
        if need_out:
            t = qg[n_ % 2]
            P.dma("sp", t[:, 0, :], T.U[c * 128:(c + 1) * 128, 768:1280])
            P.dma("act", t[:, 1, :], T.U[c * 128:(c + 1) * 128, 2304:2816])
            if c >= NTC:
                cc = cs[n_ % 2]
                t0 = (c - NTC) * 128
                P.dma("sp", cc[:, 0, :], T.c_ret_cos[t0:t0 + 128, :])
                P.dma("sp", cc[:, 1, :], T.c_ret_sin[t0:t0 + 128, :])
                rope_apply(P, Qr, t[:, 0, :], cc[:, 0, :], cc[:, 1, :], tmp, 4, 32)
                qsrc = Qr
            else:
                qsrc = t[:, 0, :]
            pq = P.bank(2, BF16)
            pk = P.bank(3, BF16)
            for h in range(4):
                P.tr(pq[:, h * 128:(h + 1) * 128], qsrc[:, h * 128:(h + 1) * 128], T.identb)
                P.tr(pk[:, h * 128:(h + 1) * 128], Krot[:, c, h * 128:(h + 1) * 128], T.identb)
            P.copy(QTs[:, 0, :], pq[:, 0:512], eng="act")
            P.tt(QTs[:, 1, :], QTs[:, 0, :], DQ[:, 0, :, :].rearrange("p h t -> p (h t)"), ALU.mult)
            P.tt(QTs[:, 2, :], QTs[:, 0, :], DQ[:, 1, :, :].rearrange("p h t -> p (h t)"), ALU.mult)
            P.copy(KTs, pk[:, 0:512], eng="act")
            if stop <= 3:
                continue
            psc = P.bank(4)
            for h in range(4):
                P.mm(psc[:, h * 128:(h + 1) * 128], KTs[:, h * 128:(h + 1) * 128], QTs[:, 0, h * 128:(h + 1) * 128])
            P.tt(AT, psc, DT.rearrange("p h t -> p (h t)"), ALU.mult)
            py = P.bank(5)
            for h in range(4):
                hs = slice(h * 128, (h + 1) * 128)
                P.mm(py[:, hs], AT[:, hs], Vs[:, c, hs], start=True, stop=False)
                P.mm(py[:, hs], QTs[:, 1, hs], Sf[:, c, hs], start=False, stop=False)
                P.mm(py[:, hs], QTs[:, 2, hs], Sbb[:, hs], start=False, stop=True)
            if stop <= 4:
                continue
            P.copy(yf, py, eng="act")
            P.tt(ysq, yf, yf, ALU.mult)
            P.reduce(rs, ysq.rearrange("p (h d) -> p h d", h=4), ALU.add)
            rsqrt_to(P, rs, rs, 1.0 / 128, EPS)
            P.tt(yf.rearrange("p (h d) -> p h d", h=4), yf.rearrange("p (h d) -> p h d", h=4),
                 rs.unsqueeze(2).to_broadcast([128, 4, 128]), ALU.mult)
            P.tt(yf, yf, gng, ALU.mult)
            P.act(sg, t[:, 1, :], AF.Silu)
            P.tt(yo, yf, sg, ALU.mult)
            if stop <= 5:
                continue
            pt = P.bank(6, BF16)
            for a in range(4):
                P.tr(pt[:, a * 128:(a + 1) * 128], yo[:, a * 128:(a + 1) * 128], T.identb)
            yt = yT[n_ % 2]
            P.copy(yt.rearrange("p a t -> p (a t)"), pt[:, 0:512], eng="act")
            P.dma("sp", T.YRT[:, :, c * 128:(c + 1) * 128].rearrange("a p t -> p a t"), yt)
        P.tt(Kd.rearrange("p (h d) -> p h d", h=4), Krot[:, c, :].rearrange("p (h d) -> p h d", h=4),
             dk[:, 1, :].unsqueeze(2).to_broadcast([128, 4, 128]), ALU.mult)
        ps = P.bank(n_ % 2)
        for h in range(4):
            P.mm(ps[:, h * 128:(h + 1) * 128], Kd[:, h * 128:(h + 1) * 128], Vs[:, c, h * 128:(h + 1) * 128])
        Sm3 = Sm.rearrange("p (h d) -> p h d", h=4)
        P.tt(Sm3, Sm3, gC[:, 4:8].unsqueeze(2).to_broadcast([128, 4, 128]), ALU.mult)
        P.tt(Sm, Sm, ps, ALU.add)
        P.copy(Sbb, Sm, eng="act")
    P.barrier()
    P.release(m0)


WDEC = 0.6065306597126334


def phase_rwkv(P, cfg, T, l, li, want_ctx):
    NT, NTC, N = cfg.NT, cfg.NTC, cfg.N
    m0 = P.mark()
    bc = lambda ap, n: ap.to_broadcast([128, n])
    mu = P.sb("mu", [128, 3, 1792], F32)
    P.dma("sp", mu[:, 1, :], bc(T.rwkv_mu[li, 0:1, :], 1792))
    P.dma("sp", mu[:, 2, :], bc(T.rwkv_mu[li, 1:2, :], 1792))
    P.tt(mu[:, 0, :], mu[:, 1, :], mu[:, 2, :], ALU.add)
    P.ts(mu[:, 0, :], mu[:, 0, :], -1.0, 1.0, ALU.mult, ALU.add)
    cv = P.sb("cv", [128, 7, 512], F32)
    P.dma("sp", cv[:, 0, :], bc(T.rwkv_k_k[li:li + 1, :], 512))
    P.dma("sp", cv[:, 1, :], bc(T.rwkv_k_a[li:li + 1, :], 512))
    P.dma("sp", cv[:, 2, :], bc(T.rwkv_r_k[li:li + 1].rearrange("o h d -> o (h d)"), 512))
    for d in range(2):
        P.dma("sp", cv[:, 3 + d, :], bc(T.rwkv_w0[li, d:d + 1, :], 512))
        P.dma("sp", cv[:, 5 + d, :], bc(T.rwkv_a0[li, d:d + 1, :], 512))
    Wl = P.sb("Wl", [128, 2, 512], BF16)
    for d in range(2):
        P.dma("pool", Wl[0:64, d, :], T.rwkv_w2[li, d])
        P.dma("pool", Wl[64:128, d, :], T.rwkv_a2[li, d])
    G2 = P.sb("G2", [128, 512], BF16)
    P.dma("pool", G2, T.rwkv_g2[li])
    mk = P.sb("rwmk", [128, 7, 128], F32)
    P.dma("sp", mk, T.c_rwmask.rearrange("p (a t) -> p a t", a=7))
    id64 = P.sb("id64", [64, 64], F32)
    P.dma("sp", id64, T.c_ident[0:64, 0:64])
    ones1 = P.sb("ones1", [128, 1], F32)
    P.memset(ones1, 1.0)
    us = [[P.sb("u%d_%d" % (a, s_), [128, 1792], BF16) for a in range(3)] for s_ in range(2)]
    tsh = P.sb("tsh", [128, 1792], F32)
    t2 = P.sb("t2", [128, 1792], F32)
    kk = P.sb("kk", [128, 512], F32)
    tA = P.sb("tA", [128, 512], F32)
    tB = P.sb("tB", [128, 512], F32)
    s8 = P.sb("s8", [128, 8], F32)
    xwag = P.sb("xwag", [128, 256], BF16)
    lT = P.sb("lT", [128, 256], BF16)
    lw = [P.sb("lw%d" % d, [128, 512], F32) for d in range(2)]
    av = [P.sb("av%d" % d, [128, 512], F32) for d in range(2)]
    krep = [P.sb("krep%d" % d, [128, 512], F32) for d in range(2)]
    bb = P.sb("bb", [128, 512], F32)
    vb = P.sb("vb", [128, 512], BF16)
    xout = P.sb("xout", [128, 2, 512], F32)
    cums = P.sb("cums", [128, 512], F32)
    pe_ = [P.sb("pe%d" % a, [128, 512], F32) for a in range(4)]
    Z = P.sb("Z", [128, 6, 512], BF16)
    ZT = P.sb("ZT", [64, 8, 4, 128], BF16)
    NN = P.sb("NN", [128, 8, 128], F32)
    LRB = P.sb("LRB", [128, 8, 128], BF16)
    AAK = P.sb("AAK", [128, 8, 128], BF16)
    LRK = P.sb("LRK", [128, 8, 128], BF16)
    AA = P.sb("AA", [128, 8, 128], F32)
    Nk = [P.sb("Nk%d" % a, [128, 8, 128], F32) for a in range(2)]
    Ak = [P.sb("Ak%d" % a, [128, 8, 128], F32) for a in range(2)]
    MT = P.sb("MT", [128, 8, 128], F32)
    MTb = P.sb("MTb", [128, 8, 128], BF16)
    ZX = P.sb("ZX", [128, 8, 128], BF16)
    WU = P.sb("WU", [128, 8, 128], BF16)
    QE = P.sb("QE", [64, 8, 128], BF16)
    Y0s = P.sb("Y0s", [128, 512], F32)
    ptot = P.sb("ptot", [64, 512], F32)
    dg = P.sb("dg", [64, 8, 64], F32)
    GTs = P.sb("GTs", [64, 512], BF16)
    H0s = P.sb("H0s", [64, 512], BF16)
    h3 = lambda a: a.rearrange("p (h d) -> p h d", h=8)
    b8 = lambda a: a.unsqueeze(2).to_broadcast([128, 8, 64])

    for i in range(NT):
        R0 = i * 128
        uc, up, un = us[i % 2]
        first = (i == 0 or i == NTC)
        last = (i == NTC - 1 or i == NT - 1)
        P.dma("sp", uc, T.U[R0:R0 + 128, 2816:4608])
        if first:
            P.memset(up[0:1, :], 0.0, eng="pool")
            P.dma("act", up[1:128, :], T.U[R0:R0 + 127, 2816:4608])
        else:
            P.dma("act", up, T.U[R0 - 1:R0 + 127, 2816:4608])
        if last:
            P.memset(un, 0.0, eng="pool")
            P.dma("act", un[0:127, :], T.U[R0 + 1:R0 + 128, 2816:4608])
        else:
            P.dma("act", un, T.U[R0 + 1:R0 + 129, 2816:4608])
        P.tt(tsh, uc, mu[:, 0, :], ALU.mult)
        P.tt(t2, up, mu[:, 1, :], ALU.mult, eng="pool")
        P.tt(tsh, tsh, t2, ALU.add)
        P.tt(t2, un, mu[:, 2, :], ALU.mult, eng="pool")
        P.tt(tsh, tsh, t2, ALU.add)
        r_, k_, v_ = tsh[:, 0:512], tsh[:, 512:1024], tsh[:, 1024:1536]
        P.tt(kk, k_, cv[:, 0, :], ALU.mult)
        P.tt(tA, kk, kk, ALU.mult)
        P.reduce(s8, h3(tA), ALU.add)
        P.ts(s8, s8, 1e-12, None, ALU.max)
        P.act(s8, s8, AF.Sqrt)
        P.recip(s8, s8)
        P.tt(h3(kk), h3(kk), b8(s8), ALU.mult)
        P.copy(vb, v_, eng="act")
        P.copy(xwag, tsh[:, 1536:1792], eng="act")
        pl = P.bank(0, BF16)
        P.tr(pl[:, 0:128], xwag[:, 0:128], T.identb)
        P.tr(pl[:, 128:256], xwag[:, 128:256], T.identb)
        P.act(lT[0:64, 0:128], pl[0:64, 0:128], AF.Tanh)
        P.act(lT[64:128, 0:128], pl[64:128, 0:128], AF.Identity)
        P.act(lT[:, 128:256], pl[:, 128:256], AF.Sigmoid)
        for d in range(2):
            pw = P.bank(1 + d)
            P.mm(pw, lT[0:64, 0:128], Wl[0:64, d, :])
            P.tt(tA, pw, cv[:, 3 + d, :], ALU.add)
            P.act(lw[d], tA, AF.Sigmoid)
            P.ts(lw[d], lw[d], -WDEC, None, ALU.mult)
            pa = P.bank(3 + d)
            P.mm(pa, lT[64:128, 0:128], Wl[64:128, d, :])
            P.tt(tB, pa, cv[:, 5 + d, :], ALU.add)
            P.act(av[d], tB, AF.Sigmoid)
            P.stt(tB, av[d], -1.0, cv[:, 1, :], ALU.add, ALU.mult)
            P.stt(krep[d], tB, 1.0, k_, ALU.add, ALU.mult)
        pg = P.bank(5)
        P.mm(pg, lT[:, 128:256], G2)
        P.copy(xout[:, 1, :], pg, eng="act")
        P.tt(tA, krep[0], krep[1], ALU.add)
        P.tt(tA, tA, r_, ALU.mult)
        P.tt(tA, tA, cv[:, 2, :], ALU.mult)
        P.reduce(s8, h3(tA), ALU.add)
        P.tt(h3(xout[:, 0, :]), h3(v_), b8(s8), ALU.mult)
        P.dma("sp", T.RWX[R0:R0 + 128, :, :], xout)
        py0 = P.bank(7)
        for d in range(2):
            P.tt(bb, kk, av[d], ALU.mult)
            pc = P.bank(6)
            P.mm(pc, mk[:, d, :], lw[d])
            P.copy(cums, pc, eng="act")
            P.mm(pc, mk[:, 2, :], lw[d])
            P.act(pe_[0], cums, AF.Exp)
            P.act(pe_[1], cums, AF.Exp, scale=-1.0)
            P.tt(tA, cums, lw[d], ALU.subtract)
            P.act(pe_[2], tA, AF.Exp)
            P.tt(tB, pc, cums, ALU.subtract)
            P.act(pe_[3], tB, AF.Exp)
            P.stt(Z[:, 0, :], kk, -1.0, pe_[2], ALU.mult, ALU.mult)
            P.tt(Z[:, 1, :], r_, pe_[0], ALU.mult)
            P.tt(Z[:, 2, :], bb, pe_[1], ALU.mult)
            P.tt(Z[:, 3, :], krep[d], pe_[1], ALU.mult, eng="pool")
            P.tt(Z[:, 4, :], bb, pe_[3], ALU.mult)
            P.tt(Z[:, 5, :], krep[d], pe_[3], ALU.mult, eng="pool")
            P.tt(ptot, pe_[3][0:64, :], pe_[0][0:64, :], ALU.mult)
            for q in range(4):
                pz = P.bank(q, BF16)
                for h in range(8):
                    P.tr(pz[0:64, h * 128:(h + 1) * 128], Z[:, q, h * 64:(h + 1) * 64], T.identb)
                P.copy(ZT[:, :, q, :], pz[0:64, 0:1024].rearrange("p (h t) -> p h t", h=8),
                       eng=("act" if q % 2 else "dve"))
            sm, im = mk[:, 3 + 2 * d, :], mk[:, 4 + 2 * d, :]
            smT = mk[:, 5 - 2 * d, :]
            mb = lambda m_: m_.unsqueeze(1).to_broadcast([128, 8, 128])
            p1 = P.bank(0, n=4)
            p1v = p1.rearrange("p (h a t) -> p h a t", h=8, a=2)
            for h in range(8):
                P.mm(p1[:, h * 256:(h + 1) * 256], ZT[:, h, 2, :], ZT[:, h, 0:2, :].rearrange("p a t -> p (a t)"))
            p3 = P.bank(4, n=2)
            for h in range(8):
                P.mm(p3[:, h * 128:(h + 1) * 128], ZT[:, h, 0, :], ZT[:, h, 2, :])
            P.tt(NN, p1v[:, :, 0, :], mb(sm), ALU.mult)
            P.tt(LRB, p1v[:, :, 1, :], mb(im), ALU.mult)
            P.tt(AA, p3.rearrange("p (h t) -> p h t", h=8), mb(smT), ALU.mult)
            for h in range(8):
                P.mm(p1[:, h * 256:(h + 1) * 256], ZT[:, h, 3, :], ZT[:, h, 0:2, :].rearrange("p a t -> p (a t)"))
            P.tt(AAK, p1v[:, :, 0, :], mb(sm), ALU.mult)
            P.tt(LRK, p1v[:, :, 1, :], mb(im), ALU.mult)
            P.tt(MT, NN, T.identf.unsqueeze(1).to_broadcast([128, 8, 128]), ALU.add)
            nk, ak = NN, AA
            for lev in range(6):
                pn = P.bank(0, n=2)
                pa2 = P.bank(2, n=2)
                for h in range(8):
                    P.mm(pn[:, h * 128:(h + 1) * 128], ak[:, h, :], nk[:, h, :])
                for h in range(8):
                    P.mm(pa2[:, h * 128:(h + 1) * 128], nk[:, h, :], ak[:, h, :])
                nk2, ak2 = Nk[lev % 2], Ak[lev % 2]
                P.copy(nk2.rearrange("p h t -> p (h t)"), pn, eng="act")
                P.copy(ak2.rearrange("p h t -> p (h t)"), pa2, eng="dve")
                nk, ak = nk2, ak2
                pm = P.bank(4, n=2)
                for h in range(8):
                    P.mm(pm[:, h * 128:(h + 1) * 128], ak[:, h, :], MT[:, h, :])
                P.tt(MT.rearrange("p h t -> p (h t)"), MT.rearrange("p h t -> p (h t)"), pm, ALU.add)
            P.copy(MTb, MT, eng="act")
            px = P.bank(0)
            for h in range(8):
                P.mm(px[:, h * 64:(h + 1) * 64], AAK[:, h, :], vb[:, h * 64:(h + 1) * 64])
            P.copy(ZX[:, :, 0:64], h3(Z[:, 0, :]), eng="pool")
            P.copy(ZX[:, :, 64:128], px.rearrange("p (h d) -> p h d", h=8), eng="act")
            pwu = P.bank(1, n=2)
            for h in range(8):
                P.mm(pwu[:, h * 128:(h + 1) * 128], MTb[:, h, :], ZX[:, h, :])
            P.copy(WU.rearrange("p h t -> p (h t)"), pwu, eng="act")
            pq = P.bank(3, n=2)
            for h in range(8):
                P.mm(pq[0:64, h * 128:(h + 1) * 128], WU[:, h, 0:64], LRB[:, h, :])
            P.tt(QE, pq[0:64, :].rearrange("p (h t) -> p h t", h=8), ZT[:, :, 1, :], ALU.add)
            P.dma("sp", T.RQE[d, i], QE.rearrange("p h t -> p (h t)"))
            for h in range(8):
                hs = slice(h * 64, (h + 1) * 64)
                P.mm(py0[:, hs], LRB[:, h, :], WU[:, h, 64:128], start=True, stop=False)
                P.mm(py0[:, hs], LRK[:, h, :], vb[:, hs], start=False, stop=True)
            if d == 0:
                P.copy(Y0s, py0, eng="act")
            else:
                P.tt(Y0s, Y0s, py0, ALU.add)
            pgt = P.bank(5)
            ph0 = P.bank(6)
            for h in range(8):
                hs = slice(h * 64, (h + 1) * 64)
                P.mm(pgt[0:64, hs], WU[:, h, 0:64], Z[:, 4, hs])
            for h in range(8):
                hs = slice(h * 64, (h + 1) * 64)
                P.mm(ph0[0:64, hs], Z[:, 4, hs], WU[:, h, 64:128], start=True, stop=False)
                P.mm(ph0[0:64, hs], Z[:, 5, hs], vb[:, hs], start=False, stop=True)
            P.tt(dg, id64.unsqueeze(1).to_broadcast([64, 8, 64]), ptot.rearrange("p (h d) -> p h d", h=8), ALU.mult)
            P.tt(GTs, pgt[0:64, :], dg.rearrange("p h d -> p (h d)"), ALU.add)
            P.copy(H0s, ph0[0:64, :], eng="act")
            P.dma("sp", T.RGT[d, i], GTs)
            P.dma("sp", T.RH0[d, i], H0s)
            if getattr(cfg, "dbg", None) == (d, i):
                for n_, tl in enumerate([LRB, LRB, AAK, LRK, LRB, MTb, ZX, WU]):
                    P.dma("sp", T.DBG[n_], tl.rearrange("p h t -> p (h t)"))
                P.dma("sp", T.DBGZ, Z.rearrange("p a c -> p (a c)"))
                P.dma("sp", T.DBGZT, ZT.rearrange("p h a t -> p (h a t)"))
        P.dma("sp", T.RY0[i], Y0s)
    P.barrier()
    P.release(m0)
    T0 = P.sb("T0", [64, 2, NT + 1, 512], BF16)
    Tm = P.sb("Tm", [64, 512], F32)
    gts = [P.sb("gts%d" % a, [64, 512], BF16) for a in range(3)]
    h0s = [P.sb("h0s%d" % a, [64, 512], BF16) for a in range(3)]
    orders = [list(range(NT)), list(range(NTC - 1, -1, -1)) + list(range(NT - 1, NTC - 1, -1))]
    slot_of = {}
    for d in range(2):
        P.memset(Tm, 0.0)
        cur = NT
        P.memset(T0[:, d, NT, :], 0.0)
        for n_, c in enumerate(orders[d]):
            slot_of[(d, c)] = cur
            g_, h_ = gts[n_ % 3], h0s[n_ % 3]
            P.dma("sp", g_, T.RGT[d, c])
            P.dma("act", h_, T.RH0[d, c])
            ps = P.bank((n_ + d) % 2)
            for h in range(8):
                hs = slice(h * 64, (h + 1) * 64)
                P.mm(ps[0:64, hs], g_[:, hs], T0[:, d, cur, hs])
            P.tt(Tm, ps[0:64, :], h_, ALU.add)
            P.copy(T0[:, d, c, :], Tm, eng="act")
            cur = c
    lng = P.sb("lng", [128, 2, 512], F32)
    P.dma("sp", lng[:, 0, :], bc(T.rwkv_ln_g[li:li + 1, :], 512))
    P.dma("sp", lng[:, 1, :], bc(T.rwkv_ln_b[li:li + 1, :], 512))
    qes = [P.sb("qes%d" % a, [64, 2, 1024], BF16) for a in range(2)]
    y0 = [P.sb("y0_%d" % a, [128, 512], F32) for a in range(2)]
    xo = [P.sb("xo%d" % a, [128, 2, 512], F32) for a in range(2)]
    y = P.sb("yw", [128, 512], F32)
    ysq = P.sb("ywsq", [128, 512], F32)
    st = P.sb("st", [128, 4, 8], F32)
    yo = P.sb("ywo", [128, 512], BF16)
    yT = [P.sb("ywT%d" % a, [128, 4, 128], BF16) for a in range(2)]
    for c in range(NT):
        if not (want_ctx or c >= NTC):
            continue
        q_, y0_, xo_ = qes[c % 2], y0[c % 2], xo[c % 2]
        P.dma("sp", q_[:, 0, :], T.RQE[0, c])
        P.dma("sp", q_[:, 1, :], T.RQE[1, c])
        P.dma("act", y0_, T.RY0[c])
        P.dma("act", xo_, T.RWX[c * 128:(c + 1) * 128, :, :])
        py = P.bank(2 + c % 2)
        for h in range(8):
            hs = slice(h * 64, (h + 1) * 64)
            for d in range(2):
                P.mm(py[:, hs], q_[:, d, h * 128:(h + 1) * 128], T0[:, d, slot_of[(d, c)], hs],
                     start=(d == 0), stop=(d == 1))
        P.tt(y, py, y0_, ALU.add)
        P.reduce(st[:, 0, :], h3(y), ALU.add)
        P.tt(ysq, y, y, ALU.mult)
        P.reduce(st[:, 1, :], h3(ysq), ALU.add)
        P.ts(st[:, 0, :], st[:, 0, :], 1.0 / 64, None, ALU.mult)
        P.tt(st[:, 2, :], st[:, 0, :], st[:, 0, :], ALU.mult)
        P.stt(st[:, 1, :], st[:, 1, :], 1.0 / 64, st[:, 2, :], ALU.mult, ALU.subtract)
        P.act(st[:, 1, :], st[:, 1, :], AF.Sqrt, bias=64e-5)
        P.recip(st[:, 1, :], st[:, 1, :])
        P.tt(h3(y), h3(y), b8(st[:, 0, :]), ALU.subtract)
        P.tt(h3(y), h3(y), b8(st[:, 1, :]), ALU.mult)
        P.tt(y, y, lng[:, 0, :], ALU.mult)
        P.tt(y, y, lng[:, 1, :], ALU.add)
        P.tt(y, y, xo_[:, 0, :], ALU.add)
        P.tt(yo, y, xo_[:, 1, :], ALU.mult)
        pt = P.bank(4 + c % 2, BF16)
        for a in range(4):
            P.tr(pt[:, a * 128:(a + 1) * 128], yo[:, a * 128:(a + 1) * 128], T.identb)
        yt = yT[c % 2]
        P.copy(yt.rearrange("p a t -> p (a t)"), pt[:, 0:512], eng="act")
        P.dma("sp", T.YWT[:, :, c * 128:(c + 1) * 128].rearrange("a p t -> p a t"), yt)
    P.barrier()
    P.release(m0)


def phase_merge(P, cfg, T, l, li):
    m0 = P.mark()
    NT, NTC = cfg.NT, cfg.NTC
    WA = P.sb("WbrA", [64, 8, D], BF16)
    WR_ = P.sb("WbrR", [128, 4, D], BF16)
    WW = P.sb("WbrW", [128, 4, D], BF16)
    WO = P.sb("WoutB", [128, 8, D], BF16)
    for h in range(8):
        P.dma("pool", WA[:, h, :], T.w_branch[li, 0, h * 64:(h + 1) * 64, :])
    for a in range(4):
        P.dma("pool", WR_[:, a, :], T.w_branch[li, 1, a * 128:(a + 1) * 128, :])
        P.dma("pool", WW[:, a, :], T.w_branch[li, 2, a * 128:(a + 1) * 128, :])
    for kt in range(8):
        P.dma("pool", WO[:, kt, :], T.w_out[li, kt * 128:(kt + 1) * 128, :])
    G1 = load_mod(P, T, l, 2, "g1")
    ya = [P.sb("mya%d" % s_, [64, 8, 128], BF16) for s_ in range(2)]
    yr = [P.sb("myr%d" % s_, [128, 4, 128], BF16) for s_ in range(2)]
    yw = [P.sb("myw%d" % s_, [128, 4, 128], BF16) for s_ in range(2)]
    gl = [P.sb("mgl%d" % s_, [128, 3 * D], BF16) for s_ in range(2)]
    gs = P.sb("mgs", [128, 3 * D], F32)
    mg = P.sb("mmg", [128, D], F32)
    tm = P.sb("mtm", [128, D], F32)
    mb = P.sb("mmb", [128, D], BF16)
    mT = P.sb("mmT", [128, 8, 128], BF16)
    xs = [P.sb("mx%d" % s_, [128, D], F32) for s_ in range(2)]
    for i in range(NT):
        cs_ = slice(i * 128, (i + 1) * 128)
        a_, r_, w_, g_, x_ = ya[i % 2], yr[i % 2], yw[i % 2], gl[i % 2], xs[i % 2]
        P.dma("sp", a_, T.YAT[:, :, cs_].rearrange("h d t -> d h t"))
        P.dma("sp", r_, T.YRT[:, :, cs_].rearrange("a p t -> p a t"))
        P.dma("sp", w_, T.YWT[:, :, cs_].rearrange("a p t -> p a t"))
        P.dma("act", g_, T.U[cs_, 4608:7680])
        P.dma("act", x_, T.XS[cs_, :])
        P.act(gs, g_, AF.Sigmoid)
        for n in range(3):
            pp = P.bank(2 * (n % 2), n=2)
            for cc in range(2):
                cols = slice(cc * 512, (cc + 1) * 512)
                if n == 0:
                    for h in range(8):
                        P.mm(pp[:, cols], a_[:, h, :], WA[:, h, cols], start=(h == 0), stop=(h == 7))
                else:
                    src, wt = (r_, WR_) if n == 1 else (w_, WW)
                    for a in range(4):
                        P.mm(pp[:, cols], src[:, a, :], wt[:, a, cols], start=(a == 0), stop=(a == 3))
            if n == 0:
                P.tt(mg, pp, gs[:, 0:D], ALU.mult)
            else:
                P.tt(tm, pp, gs[:, n * D:(n + 1) * D], ALU.mult)
                P.tt(mg, mg, tm, ALU.add)
        P.copy(mb, mg, eng="act")
        pt = P.bank(6, BF16)
        for kt in range(8):
            P.tr(pt[:, kt * 128:(kt + 1) * 128], mb[:, kt * 128:(kt + 1) * 128], T.identb)
        P.copy(mT.rearrange("p k t -> p (k t)"), pt[:, 0:1024], eng="act")
        po = P.bank(4, n=2)
        for cc in range(2):
            cols = slice(cc * 512, (cc + 1) * 512)
            for kt in range(8):
                P.mm(po[:, cols], mT[:, kt, :], WO[:, kt, cols], start=(kt == 0), stop=(kt == 7))
        P.tt(tm, po, G1[0 if i < NTC else 1], ALU.mult)
        P.tt(x_, x_, tm, ALU.add)
        P.dma("sp", T.XM[cs_, :], x_)
    P.barrier()
    P.release(m0)


def phase_moe(P, cfg, T, l, li):
    m0 = P.mark()
    NT, NTC, N = cfg.NT, cfg.NTC, cfg.N
    Wr = P.sb("Wr", [128, 8, 36], F32)
    P.dma("sp", Wr[:, :, 0:4], T.router_grp_w[li].rearrange("(kt p) c -> p kt c", p=128))
    P.dma("sp", Wr[:, :, 4:36], T.router_exp_w[li].rearrange("(kt p) c -> p kt c", p=128))
    rb = P.sb("rb", [128, 36], F32)
    P.dma("sp", rb[:, 0:4], T.router_grp_b[li:li + 1, :].to_broadcast([128, 4]))
    P.dma("sp", rb[:, 4:36], T.router_exp_b[li:li + 1, :].to_broadcast([128, 32]))
    WR = P.sb("WRt", [128, NT, 32], F32)
    mA = P.mark()
    G2 = load_mod(P, T, l, 4, "G2")
    SH2 = load_mod(P, T, l, 3, "SH2")
    xs = [P.sb("ex%d" % s_, [128, D], F32) for s_ in range(2)]
    hf = P.sb("ehf", [128, D], F32)
    hbj = P.sb("ehb", [128, D], BF16)
    h2Tf = P.sb("eh2Tf", [128, 8, 128], F32)
    h2Tb = [P.sb("eh2Tb%d" % s_, [128, 8, 128], BF16) for s_ in range(2)]
    ss = P.sb("ess", [128, 1], F32)
    rstd = P.sb("erstd", [128, 1], F32)
    lg = P.sb("elg", [128, 36], F32)
    sm = P.sb("esm", [128, 16], F32)
    gmask = P.sb("egm", [128, 4], F32)
    e4 = P.sb("ee4", [128, 4], F32)
    sel = P.sb("esel", [128, 4, 8], F32)
    elg = P.sb("eelg", [128, 8], F32)
    srt = P.sb("esrt", [128, 8], F32)
    m8 = P.sb("em8", [128, 8], F32)
    e8 = P.sb("ee8", [128, 8], F32)
    for i in range(NT):
        cs_ = slice(i * 128, (i + 1) * 128)
        x_ = xs[i % 2]
        isl = 0 if i < NTC else 1
        import os
        stp = os.environ.get("MOE_A", "9")
        if stp == "0":
            continue
        P.dma("sp", x_, T.XM[cs_, :])
        rms_mod_tile(P, x_, G2[isl], SH2[isl], hf, hbj, ss, rstd)
        P.tt(hf, hf, SH2[isl], ALU.add)
        if stp == "0.5":
            continue
        ptf = P.bank(0, n=2)
        for kt in range(8):
            P.tr(ptf[:, kt * 128:(kt + 1) * 128], hf[:, kt * 128:(kt + 1) * 128], T.identf)
        P.copy(h2Tf.rearrange("p k t -> p (k t)"), ptf, eng="act")
        hb = h2Tb[i % 2]
        P.copy(hb.rearrange("p k t -> p (k t)"), h2Tf.rearrange("p k t -> p (k t)"), eng="dve")
        P.dma("sp", T.H2T[:, :, cs_], hb)
        import os
        stp = os.environ.get("MOE_A", "9")
        if stp == "1":
            continue
        pl = P.bank(2)
        for kt in range(8):
            P.mm(pl[:, 0:36], h2Tf[:, kt, :], Wr[:, kt, :], start=(kt == 0), stop=(kt == 7))
        P.tt(lg, pl[:, 0:36], rb, ALU.add)
        P.reduce(sm[:, 0:1], lg[:, 0:4], ALU.max)
        P.ts(gmask, lg[:, 0:4], sm[:, 0:1], None, ALU.is_equal)
        P.ts(sm[:, 1:2], sm[:, 0:1], -1.0, None, ALU.mult)
        P.act(e4, lg[:, 0:4], AF.Exp, bias=sm[:, 1:2])
        P.reduce(sm[:, 2:3], e4, ALU.add)
        P.recip(sm[:, 2:3], sm[:, 2:3])
        if stp == "2":
            continue
        el = lg[:, 4:36].rearrange("p (g e) -> p g e", g=4)
        P.tt(sel, el, gmask.unsqueeze(2).to_broadcast([128, 4, 8]), ALU.mult)
        P.reduce(elg, sel.rearrange("p g e -> p e g"), ALU.add)
        P.generic("dve", (lambda e, o_=_ap(srt), i_=_ap(elg): e.max(o_, i_)), [elg], [srt])
        P.ts(m8, elg, srt[:, 1:2], None, ALU.is_ge)
        P.ts(sm[:, 3:4], srt[:, 0:1], -1.0, None, ALU.mult)
        P.act(e8, elg, AF.Exp, bias=sm[:, 3:4])
        P.tt(e8, e8, m8, ALU.mult)
        P.reduce(sm[:, 4:5], e8, ALU.add)
        P.recip(sm[:, 4:5], sm[:, 4:5])
        P.tt(sm[:, 5:6], sm[:, 4:5], sm[:, 2:3], ALU.mult)
        P.ts(e8, e8, sm[:, 5:6], None, ALU.mult)
        P.tt(WR[:, i, :].rearrange("p (g e) -> p g e", g=4), gmask.unsqueeze(2).to_broadcast([128, 4, 8]),
             e8.unsqueeze(1).to_broadcast([128, 4, 8]), ALU.mult)
    P.barrier()
    P.release(mA)
    import os
    if os.environ.get("MOE_STOP") == "1":
        P.release(m0)
        return
    TH = 17
    g2m = load_mod(P, T, l, 5, "g2m")
    wg = [P.sb("wg%d" % s_, [128, 8, 512], BF16) for s_ in range(2)]
    wu = [P.sb("wu%d" % s_, [128, 8, 512], BF16) for s_ in range(2)]
    wd = [P.sb("wd%d" % s_, [128, 4, D], BF16) for s_ in range(2)]
    nth = min(TH, NT)
    H2 = P.sb("H2", [128, 8, nth * 128], BF16)
    yacc = P.sb("yacc", [128, nth, D], F32)
    HT = P.sb("HT", [128, 4, nth * 128], BF16)
    sg = [P.sb("esg%d" % s_, [128, 512], F32) for s_ in range(2)]
    xo = [P.sb("exo%d" % s_, [128, D], F32) for s_ in range(2)]
    ne = 0
    for t0 in range(0, NT, TH):
        nt = min(TH, NT - t0)
        ntok = nt * 128
        P.dma("sp", H2[:, :, 0:ntok], T.H2T[:, :, t0 * 128:t0 * 128 + ntok])
        P.memset(yacc[:, 0:nt, :], 0.0, eng="pool")
        for e in range(32):
            g_, u_, d_ = wg[ne % 2], wu[ne % 2], wd[ne % 2]
            ne += 1
            gv = T.moe_w_gate[li, e].rearrange("(kt p) c -> p kt c", p=128)
            uv = T.moe_w_up[li, e].rearrange("(kt p) c -> p kt c", p=128)
            dv = T.moe_w_down[li, e].rearrange("(a p) c -> p a c", p=128)
            for kt in range(8):
                P.dma("pool", g_[:, kt, :], gv[:, kt, :])
                P.dma("pool", u_[:, kt, :], uv[:, kt, :])
            for a in range(4):
                P.dma("pool", d_[:, a, :], dv[:, a, :])
            nb = 0
            for ht in range(4):
                hsl = slice(ht * 128, (ht + 1) * 128)
                for c0 in range(0, ntok, 512):
                    cw = min(512, ntok - c0)
                    pg = P.bank(2 * (nb % 2))
                    pu = P.bank(2 * (nb % 2) + 1)
                    s_ = sg[nb % 2]
                    nb += 1
                    for kt in range(8):
                        P.mm(pg[:, 0:cw], g_[:, kt, hsl], H2[:, kt, c0:c0 + cw], start=(kt == 0), stop=(kt == 7))
                    for kt in range(8):
                        P.mm(pu[:, 0:cw], u_[:, kt, hsl], H2[:, kt, c0:c0 + cw], start=(kt == 0), stop=(kt == 7))
                    P.act(s_[:, 0:cw], pg[:, 0:cw], AF.Silu)
                    P.tt(HT[:, ht, c0:c0 + cw], s_[:, 0:cw], pu[:, 0:cw], ALU.mult)
            for j in range(nt):
                po = P.bank(4 + 2 * (j % 2), n=2)
                for cc in range(2):
                    cols = slice(cc * 512, (cc + 1) * 512)
                    for a in range(4):
                        P.mm(po[:, cols], HT[:, a, j * 128:(j + 1) * 128], d_[:, a, cols], start=(a == 0), stop=(a == 3))
                P.stt(yacc[:, j, :], po, WR[:, t0 + j, e:e + 1], yacc[:, j, :], ALU.mult, ALU.add)
        for j in range(nt):
            i = t0 + j
            cs_ = slice(i * 128, (i + 1) * 128)
            x_ = xo[j % 2]
            P.dma("act", x_, T.XM[cs_, :])
            P.tt(yacc[:, j, :], yacc[:, j, :], g2m[0 if i < NTC else 1], ALU.mult)
            P.tt(x_, x_, yacc[:, j, :], ALU.add)
            P.dma("sp", T.XS[cs_, :], x_)
    P.barrier()
    P.release(m0)


def build(cfg, phases=("ada", "inproj"), debug_outs=()):
    nc = bass.Bass("TRN2", target_bir_lowering=False)
    P = Prog(nc)
    T = Ctx()
    L = len(cfg.layers)
    T.x = nc.dram_tensor("x", [cfg.NL, D], F32, kind="ExternalInput").ap()
    T.ctx = nc.dram_tensor("ctx", [cfg.NC, D], F32, kind="ExternalInput").ap()
    T.c = nc.dram_tensor("c", [1, D], F32, kind="ExternalInput").ap()
    T.c_ctx = nc.dram_tensor("c_ctx", [1, D], F32, kind="ExternalInput").ap()
    for name, shp in WEIGHT_SPECS:
        setattr(T, name, nc.dram_tensor(name, [L] + list(shp), F32, kind="ExternalInput").ap())
    hc = host_consts(cfg)
    for name, arr in hc.items():
        setattr(T, "c_" + name, nc.dram_tensor("c_" + name, list(arr.shape), F32, kind="ExternalInput").ap())

    def scratch(name, shape, dt):
        kind = "ExternalOutput" if name in debug_outs else "Internal"
        return nc.dram_tensor(name, list(shape), dt, kind=kind).ap()

    T.XS = scratch("XS", [cfg.N, D], F32)
    T.MOD = scratch("MOD", [cfg.LT, 2, 6 * D], F32)
    T.U = scratch("U", [cfg.N, IN_COLS], BF16)
    T.YAT = scratch("YAT", [8, 64, cfg.N], BF16)
    T.YRT = scratch("YRT", [4, 128, cfg.N], BF16)
    T.YWT = scratch("YWT", [4, 128, cfg.N], BF16)
    T.RWX = scratch("RWX", [cfg.N, 2, 512], F32)
    T.RQE = scratch("RQE", [2, cfg.NT, 64, 1024], BF16)
    T.RY0 = scratch("RY0", [cfg.NT, 128, 512], F32)
    T.RGT = scratch("RGT", [2, cfg.NT, 64, 512], BF16)
    T.RH0 = scratch("RH0", [2, cfg.NT, 64, 512], BF16)
    T.DBG = scratch("DBG", [8, 128, 1024], BF16)
    T.DBGZ = scratch("DBGZ", [128, 3072], BF16)
    T.DBGZT = scratch("DBGZT", [64, 4096], BF16)
    T.XM = scratch("XM", [cfg.N, D], F32)
    T.H2T = scratch("H2T", [128, 8, cfg.N], BF16)
    T.out = nc.dram_tensor("out", [cfg.NL, D], F32, kind="ExternalOutput").ap()
    T.ctx_out = nc.dram_tensor("ctx_out", [cfg.NC, D], F32, kind="ExternalOutput").ap()

    T.identf = P.sb("identf", [128, 128], F32)
    T.identb = P.sb("identb", [128, 128], BF16)
    P.dma("sp", T.identf, T.c_ident)
    P.dma("pool", T.identb, T.c_ident)
    phase0(P, cfg, T)
    for li, l in enumerate(cfg.layers):
        if "ada" in phases:
            phase_ada(P, cfg, T, l, li)
        if "inproj" in phases:
            phase_inproj(P, cfg, T, l, li)
        want_ctx = (l < cfg.LT - 1) or cfg.want_ctx_last
        if "attn" in phases:
            phase_attn(P, cfg, T, l, li, want_ctx)
        if "ret" in phases:
            phase_ret(P, cfg, T, l, li, want_ctx)
        if "rwkv" in phases:
            phase_rwkv(P, cfg, T, l, li, want_ctx)
        if "merge" in phases:
            phase_merge(P, cfg, T, l, li)
        if "moe" in phases:
            phase_moe(P, cfg, T, l, li)
    P.dma("sp", T.out, T.XS[cfg.NC:cfg.N, :])
    P.dma("sp", T.ctx_out, T.XS[0:cfg.NC, :])
    fk = ["out", "ctx_out"] + list(debug_outs)
    P.emit(final_keys=fk)
    return nc, P


FUSED = True
ALL_PHASES = ("ada", "inproj", "attn", "ret", "rwkv", "merge", "moe")
_PROG_CACHE = {}


def _get_prog(key, cfg):
    if key not in _PROG_CACHE:
        _PROG_CACHE[key] = build(cfg, phases=ALL_PHASES)[0]
    return _PROG_CACHE[key]


def kernel(**inputs):
    from concourse.bass_utils import run_bass_kernel_spmd
    inp = {k: np.ascontiguousarray(np.asarray(v, dtype=np.float32)) for k, v in inputs.items()}
    B, NL, _ = inp["x"].shape
    NC = inp["ctx"].shape[1]
    depth = inp["w_in"].shape[0]
    ncores = B
    if FUSED:
        cfg = Cfg(NL=NL, NC=NC, layers=list(range(depth)), nlayers_total=depth, want_ctx_last=False)
        launches = [list(range(depth))]
    else:
        cfg = Cfg(NL=NL, NC=NC, layers=[0], nlayers_total=1, want_ctx_last=True)
        launches = [[l] for l in range(depth)]
    nc = _get_prog(("fused" if FUSED else "layer", NL, NC, depth), cfg)
    consts = {"c_" + k: v for k, v in host_consts(cfg).items()}
    xs = [inp["x"][b] for b in range(B)]
    cx = [inp["ctx"][b] for b in range(B)]
    for ls in launches:
        wl = {name: np.ascontiguousarray(inp[name][ls]) for name, _ in WEIGHT_SPECS}
        in_maps = []
        for b in range(ncores):
            m = dict(x=xs[b], ctx=cx[b], c=inp["c"][b:b + 1], c_ctx=inp["c_ctx"][None, :])
            m.update(wl)
            m.update(consts)
            in_maps.append(m)
        res = run_bass_kernel_spmd(nc, in_maps, core_ids=list(range(ncores)))
        xs = [np.asarray(r["out"]) for r in res.results]
        cx = [np.asarray(r["ctx_out"]) for r in res.results]
    return np.stack(xs).astype(np.float32)
```
